# Optimizing a Trainium2 kernel written in Bass

```python
import math
import jax, jax.numpy as jnp
from jax import lax
import numpy as np

D_MODEL = 4096
BATCH = 2
SEQ = 4096
DEPTH = 1

N_META = 16
CHUNK = 128
Q_BLOCK = 128
PAD_FRONT = CHUNK - N_META
MIX_WIDTH = D_MODEL
ATTN_WIDTH = MIX_WIDTH // 2
SSM_WIDTH = MIX_WIDTH - ATTN_WIDTH
ATTN_HEAD_DIM = 128
ATTN_HEADS = ATTN_WIDTH // (2 * ATTN_HEAD_DIM)
N_BUCKETS = 32
MAX_DISTANCE = 128
SSM_HEAD_DIM = 64
SSM_HEADS = SSM_WIDTH // SSM_HEAD_DIM
SSM_STATE = 128
SSM_GROUPS = 8
HEADS_PER_GROUP = SSM_HEADS // SSM_GROUPS
CONV_WIDTH = 4
CONV_CH = SSM_WIDTH + 2 * SSM_GROUPS * SSM_STATE
DT_MIN = 0.001
DT_MAX = 0.1
N_EXPERT_GROUPS = 8
EXPERTS_PER_GROUP = 8
N_EXPERTS = N_EXPERT_GROUPS * EXPERTS_PER_GROUP
TOP_K = 2
D_EXPERT = 768
MOE_BLOCK = 128
EPS = 1e-6
NEG = -1e30
Q_SIZE = 2 * ATTN_HEADS * ATTN_HEAD_DIM
V_SIZE = ATTN_HEADS * 2 * ATTN_HEAD_DIM
BC_SIZE = SSM_GROUPS * SSM_STATE
OFF_K = Q_SIZE
OFF_V = OFF_K + Q_SIZE
OFF_Z = OFF_V + V_SIZE
OFF_X = OFF_Z + SSM_WIDTH
OFF_B = OFF_X + SSM_WIDTH
OFF_C = OFF_B + BC_SIZE
OFF_DT = OFF_C + BC_SIZE
N_IN = OFF_DT + SSM_HEADS

kernel_name = 'hymba_diffattn_ssd_hiermoe'


def rms_norm(u, w):
    uf = u.astype(jnp.float32)
    y = uf * lax.rsqrt(jnp.mean(uf * uf, axis=-1, keepdims=True) + EPS)
    return (y * w.astype(jnp.float32)).astype(u.dtype)


def gated_group_rms_norm(y, z, w):
    g = (y * jax.nn.silu(z)).astype(jnp.float32)
    shp = g.shape
    g = g.reshape(shp[:-1] + (SSM_GROUPS, shp[-1] // SSM_GROUPS))
    g = g * lax.rsqrt(jnp.mean(g * g, axis=-1, keepdims=True) + EPS)
    return (g.reshape(shp) * w.astype(jnp.float32)).astype(y.dtype)


def pad_front(t, n):
    return jnp.pad(t, [(0, 0), (n, 0)] + [(0, 0)] * (t.ndim - 2))


def t5_bucket(rel):
    n = jnp.maximum(rel, 0)
    max_exact = N_BUCKETS // 2
    nf = jnp.maximum(n, 1).astype(jnp.float32)
    large = max_exact + (jnp.log(nf / max_exact) / math.log(MAX_DISTANCE / max_exact)
                         * (N_BUCKETS - max_exact)).astype(jnp.int32)
    large = jnp.minimum(large, N_BUCKETS - 1)
    return jnp.where(n < max_exact, n, large)


def diff_attention(q, k, v, rel_bias, lam):
    bsz, lp = q.shape[0], q.shape[1]
    n_blocks = lp // Q_BLOCK
    k_pos = jnp.arange(lp)
    scale = ATTN_HEAD_DIM ** -0.5

    def block(i):
        q0 = i * Q_BLOCK
        qb = lax.dynamic_slice_in_dim(q, q0, Q_BLOCK, axis=1)
        s = jnp.einsum('bqhd,bkhd->bhqk', qb, k, preferred_element_type=jnp.float32) * scale
        s = s.reshape(bsz, ATTN_HEADS, 2, Q_BLOCK, lp)
        q_pos = q0 + jnp.arange(Q_BLOCK)
        rel = q_pos[:, None] - k_pos[None, :]
        bias = jnp.moveaxis(rel_bias[t5_bucket(rel)], -1, 0).astype(jnp.float32)
        mask = (rel >= 0) & (k_pos[None, :] >= PAD_FRONT)
        s = jnp.where(mask, s + bias[None, :, None], NEG)
        p = jax.nn.softmax(s, axis=-1)
        a = (p[:, :, 0] - lam * p[:, :, 1]).astype(v.dtype)
        return jnp.einsum('bhqk,bkhe->bqhe', a, v)

    out = lax.map(block, jnp.arange(n_blocks))
    return jnp.moveaxis(out, 0, 1).reshape(bsz, lp, ATTN_HEADS, 2 * ATTN_HEAD_DIM)


def segsum(a):
    t = a.shape[-1]
    x = jnp.broadcast_to(a[..., None], a.shape + (t,))
    x = jnp.where(jnp.tril(jnp.ones((t, t), bool), -1), x, 0.0)
    s = jnp.cumsum(x, axis=-2)
    return jnp.where(jnp.tril(jnp.ones((t, t), bool)), s, -jnp.inf)


def ssd_chunked(xh, dt, a, bm, cm):
    bsz, lp, nh, hp = xh.shape
    nc = lp // CHUNK
    dtype = xh.dtype
    X = (xh * dt[..., None].astype(dtype)).reshape(bsz, nc, CHUNK, SSM_GROUPS, HEADS_PER_GROUP, hp)
    a_dt = (dt * a).reshape(bsz, nc, CHUNK, SSM_GROUPS, HEADS_PER_GROUP)
    a_dt = jnp.moveaxis(a_dt, 2, -1)
    a_cs = jnp.cumsum(a_dt, axis=-1)
    Bc = bm.reshape(bsz, nc, CHUNK, SSM_GROUPS, SSM_STATE)
    Cc = cm.reshape(bsz, nc, CHUNK, SSM_GROUPS, SSM_STATE)
    decay_in = jnp.exp(segsum(a_dt)).astype(dtype)
    cb = jnp.einsum('bclgn,bcsgn->bcgls', Cc, Bc)
    y_diag = jnp.einsum('bcgrls,bcsgrp->bclgrp', cb[:, :, :, None] * decay_in, X)
    decay_to_end = jnp.exp(a_cs[..., -1:] - a_cs).astype(dtype)
    states = jnp.einsum('bclgn,bcgrl,bclgrp->bcgrpn', Bc, decay_to_end, X)
    chunk_decay = jnp.exp(a_cs[..., -1]).astype(dtype)

    def step(hc, inp):
        s_c, d_c = inp
        return hc * d_c[..., None, None] + s_c, hc

    h0 = jnp.zeros_like(states[:, 0])
    _, h_in = lax.scan(step, h0, (jnp.moveaxis(states, 1, 0), jnp.moveaxis(chunk_decay, 1, 0)))
    h_in = jnp.moveaxis(h_in, 0, 1)
    y_off = jnp.einsum('bclgn,bcgrpn,bcgrl->bclgrp', Cc, h_in, jnp.exp(a_cs).astype(dtype))
    return (y_diag + y_off).reshape(bsz, lp, nh, hp)


def hybrid_mixer(u, rel_bias, w_in, conv_w, conv_b, dt_bias, a_log, d_skip, ssm_norm_w,
                 lq1, lk1, lq2, lk2, subln_w, w_out, lambda_init):
    bsz, L, _ = u.shape
    lp = L + PAD_FRONT
    proj = jnp.einsum('bld,dn->bln', u, w_in)
    q = pad_front(proj[..., :OFF_K].reshape(bsz, L, 2 * ATTN_HEADS, ATTN_HEAD_DIM), PAD_FRONT)
    k = pad_front(proj[..., OFF_K:OFF_V].reshape(bsz, L, 2 * ATTN_HEADS, ATTN_HEAD_DIM), PAD_FRONT)
    v = pad_front(proj[..., OFF_V:OFF_Z].reshape(bsz, L, ATTN_HEADS, 2 * ATTN_HEAD_DIM), PAD_FRONT)
    f32 = jnp.float32
    lam = (jnp.exp(jnp.sum(lq1.astype(f32) * lk1.astype(f32)))
           - jnp.exp(jnp.sum(lq2.astype(f32) * lk2.astype(f32))) + lambda_init)
    attn = diff_attention(q, k, v, rel_bias, lam)[:, PAD_FRONT:]
    attn = (rms_norm(attn, subln_w) * (1.0 - lambda_init)).reshape(bsz, L, ATTN_WIDTH)
    z = proj[..., OFF_Z:OFF_X]
    xbc = proj[..., OFF_X:OFF_DT]
    xbc = jnp.pad(xbc, ((0, 0), (PAD_FRONT + CONV_WIDTH - 1, 0), (0, 0)))
    conv = conv_b
    for j in range(CONV_WIDTH):
        conv = conv + xbc[:, j:j + lp] * conv_w[j]
    xbc = jax.nn.silu(conv)
    xh = xbc[..., :SSM_WIDTH].reshape(bsz, lp, SSM_HEADS, SSM_HEAD_DIM)
    bm = xbc[..., SSM_WIDTH:SSM_WIDTH + BC_SIZE].reshape(bsz, lp, SSM_GROUPS, SSM_STATE)
    cm = xbc[..., SSM_WIDTH + BC_SIZE:].reshape(bsz, lp, SSM_GROUPS, SSM_STATE)
    dt = jax.nn.softplus(proj[..., OFF_DT:].astype(f32) + dt_bias.astype(f32))
    dt = pad_front(dt, PAD_FRONT)
    a = -jnp.exp(a_log.astype(f32))
    y = ssd_chunked(xh, dt, a, bm, cm) + xh * d_skip[:, None].astype(xh.dtype)
    y = y[:, PAD_FRONT:].reshape(bsz, L, SSM_WIDTH)
    ssm = gated_group_rms_norm(y, z, ssm_norm_w)
    mixed = jnp.concatenate([attn, ssm], axis=-1)
    return jnp.einsum('blm,md->bld', mixed, w_out)


def block_sparse_experts(tok, expert_ids, weights, w_gate, w_up, w_down):
    T, D = tok.shape
    A = T * TOP_K
    flat_e = expert_ids.reshape(-1)
    flat_t = jnp.repeat(jnp.arange(T, dtype=jnp.int32), TOP_K)
    flat_w = weights.reshape(-1)
    order = jnp.argsort(flat_e)
    se, st, sw = flat_e[order], flat_t[order], flat_w[order]
    counts = jnp.bincount(flat_e, length=N_EXPERTS)
    starts = jnp.cumsum(counts) - counts
    pcounts = (counts + MOE_BLOCK - 1) // MOE_BLOCK * MOE_BLOCK
    pends = jnp.cumsum(pcounts)
    pstarts = pends - pcounts
    dest = pstarts[se] + (jnp.arange(A) - starts[se])
    n_blocks = (A + N_EXPERTS * (MOE_BLOCK - 1) + MOE_BLOCK - 1) // MOE_BLOCK
    rows = n_blocks * MOE_BLOCK
    row_tok = jnp.full((rows,), T, jnp.int32).at[dest].set(st)
    row_w = jnp.zeros((rows,), tok.dtype).at[dest].set(sw.astype(tok.dtype))
    block_e = jnp.minimum(jnp.searchsorted(pends, jnp.arange(n_blocks) * MOE_BLOCK, side='right'),
                          N_EXPERTS - 1)
    tok_pad = jnp.concatenate([tok, jnp.zeros((1, D), tok.dtype)], axis=0)

    def run_block(args):
        idx, wr, e = args
        xb = tok_pad[idx]
        hdn = jax.nn.silu(xb @ w_gate[e]) * (xb @ w_up[e])
        return (hdn @ w_down[e]) * wr[:, None]

    ys = lax.map(run_block, (row_tok.reshape(n_blocks, MOE_BLOCK),
                             row_w.reshape(n_blocks, MOE_BLOCK), block_e))
    out = jnp.zeros((T + 1, D), tok.dtype).at[row_tok].add(ys.reshape(rows, D))
    return out[:T]


def hier_moe(u, wg, bg, we, be, w_gate, w_up, w_down):
    bsz, L, D = u.shape
    tok = u.reshape(-1, D)
    T = tok.shape[0]
    f32 = jnp.float32
    g_logits = jnp.einsum('td,dg->tg', tok, wg, preferred_element_type=f32) + bg.astype(f32)
    g_prob = jax.nn.softmax(g_logits, axis=-1)
    g_sel = jnp.argmax(g_logits, axis=-1).astype(jnp.int32)
    g_w = jnp.take_along_axis(g_prob, g_sel[:, None], axis=1)
    e_logits = (jnp.einsum('td,de->te', tok, we, preferred_element_type=f32)
                + be.astype(f32)).reshape(T, N_EXPERT_GROUPS, EXPERTS_PER_GROUP)
    e_logits = jnp.take_along_axis(e_logits, g_sel[:, None, None], axis=1)[:, 0]
    e_prob = jax.nn.softmax(e_logits, axis=-1)
    top_p, top_i = lax.top_k(e_prob, TOP_K)
    weights = g_w * top_p / jnp.sum(top_p, axis=-1, keepdims=True)
    expert_ids = g_sel[:, None] * EXPERTS_PER_GROUP + top_i.astype(jnp.int32)
    out = block_sparse_experts(tok, expert_ids, weights, w_gate, w_up, w_down)
    return out.reshape(bsz, L, D)


def setup_inputs(seed: int = 0) -> dict:
    key = jax.random.key(seed)
    ks = jax.random.split(key, 26)
    f32 = jnp.float32

    def nrm(k, shape, scale):
        return scale * jax.random.normal(k, shape, f32)

    dt0 = jnp.exp(jax.random.uniform(ks[7], (DEPTH, SSM_HEADS), f32, math.log(DT_MIN), math.log(DT_MAX)))
    return {
        'x': nrm(ks[0], (BATCH, SEQ, D_MODEL), 1.0),
        'meta_tokens': nrm(ks[1], (N_META, D_MODEL), 1.0),
        'rel_bias': nrm(ks[2], (N_BUCKETS, ATTN_HEADS), 0.5),
        'norm1_w': 1.0 + nrm(ks[3], (DEPTH, D_MODEL), 0.02),
        'w_in': nrm(ks[4], (DEPTH, D_MODEL, N_IN), D_MODEL ** -0.5),
        'conv_w': nrm(ks[5], (DEPTH, CONV_WIDTH, CONV_CH), CONV_WIDTH ** -0.5),
        'conv_b': nrm(ks[6], (DEPTH, CONV_CH), 0.02),
        'dt_bias': dt0 + jnp.log(-jnp.expm1(-dt0)),
        'a_log': jnp.log(jax.random.uniform(ks[8], (DEPTH, SSM_HEADS), f32, 1.0, 16.0)),
        'd_skip': 1.0 + nrm(ks[9], (DEPTH, SSM_HEADS), 0.02),
        'ssm_norm_w': 1.0 + nrm(ks[10], (DEPTH, SSM_WIDTH), 0.02),
        'lambda_q1': nrm(ks[11], (DEPTH, ATTN_HEAD_DIM), 0.1),
        'lambda_k1': nrm(ks[12], (DEPTH, ATTN_HEAD_DIM), 0.1),
        'lambda_q2': nrm(ks[13], (DEPTH, ATTN_HEAD_DIM), 0.1),
        'lambda_k2': nrm(ks[14], (DEPTH, ATTN_HEAD_DIM), 0.1),
        'subln_w': 1.0 + nrm(ks[15], (DEPTH, 2 * ATTN_HEAD_DIM), 0.02),
        'w_out': nrm(ks[16], (DEPTH, MIX_WIDTH, D_MODEL), MIX_WIDTH ** -0.5),
        'norm2_w': 1.0 + nrm(ks[17], (DEPTH, D_MODEL), 0.02),
        'router_group_w': nrm(ks[18], (DEPTH, D_MODEL, N_EXPERT_GROUPS), D_MODEL ** -0.5),
        'router_group_b': nrm(ks[19], (DEPTH, N_EXPERT_GROUPS), 0.01),
        'router_expert_w': nrm(ks[20], (DEPTH, D_MODEL, N_EXPERTS), D_MODEL ** -0.5),
        'router_expert_b': nrm(ks[21], (DEPTH, N_EXPERTS), 0.01),
        'expert_w_gate': nrm(ks[22], (DEPTH, N_EXPERTS, D_MODEL, D_EXPERT), D_MODEL ** -0.5),
        'expert_w_up': nrm(ks[23], (DEPTH, N_EXPERTS, D_MODEL, D_EXPERT), D_MODEL ** -0.5),
        'expert_w_down': nrm(ks[24], (DEPTH, N_EXPERTS, D_EXPERT, D_MODEL), D_EXPERT ** -0.5),
        'final_norm_w': 1.0 + nrm(ks[25], (D_MODEL,), 0.02),
    }


def reference(x, meta_tokens, rel_bias, norm1_w, w_in, conv_w, conv_b, dt_bias, a_log, d_skip,
              ssm_norm_w, lambda_q1, lambda_k1, lambda_q2, lambda_k2, subln_w, w_out, norm2_w,
              router_group_w, router_group_b, router_expert_w, router_expert_b,
              expert_w_gate, expert_w_up, expert_w_down, final_norm_w):
    bsz = x.shape[0]
    meta = jnp.broadcast_to(meta_tokens[None].astype(x.dtype), (bsz, N_META, D_MODEL))
    h = jnp.concatenate([meta, x], axis=1)
    for layer in range(DEPTH):
        lambda_init = 0.8 - 0.6 * math.exp(-0.3 * layer)
        u = rms_norm(h, norm1_w[layer])
        h = h + hybrid_mixer(u, rel_bias, w_in[layer], conv_w[layer], conv_b[layer], dt_bias[layer],
                             a_log[layer], d_skip[layer], ssm_norm_w[layer], lambda_q1[layer],
                             lambda_k1[layer], lambda_q2[layer], lambda_k2[layer], subln_w[layer],
                             w_out[layer], lambda_init)
        u = rms_norm(h, norm2_w[layer])
        h = h + hier_moe(u, router_group_w[layer], router_group_b[layer], router_expert_w[layer],
                         router_expert_b[layer], expert_w_gate[layer], expert_w_up[layer],
                         expert_w_down[layer])
    h = rms_norm(h, final_norm_w)
    return h[:, N_META:]
```

```python
import numpy as np
import ml_dtypes
from contextlib import ExitStack

import concourse.bass as bass
import concourse.mybir as mybir
from concourse.bass_utils import run_bass_kernel_spmd

F32 = mybir.dt.float32
BF16 = mybir.dt.bfloat16
I32 = mybir.dt.int32
AF = mybir.ActivationFunctionType
ALU = mybir.AluOpType
AX = mybir.AxisListType

NCORES = 8
D = 4096
SEQ = 4096
NB = 2
TPB = 33
NT = NB * TPB
NTOK = NT * 128
NCOL = 1540
EPS = 1e-6
NEG = -1e30
SCALE = 128 ** -0.5
LAMBDA_INIT = 0.2
DBG = set()
SSD_LIM = 99
S7 = 99


class Stream:
    def __init__(self, name, sem, kind, step=16):
        self.name, self.sem, self.kind, self.count, self.step = name, sem, kind, 0, step


class Res:
    __slots__ = ("name", "w", "rs")

    def __init__(self, name=""):
        self.name = name
        self.w = None
        self.rs = {}


ENGS = ("pe", "act", "dve", "pool", "sp")


class Prog:
    def __init__(self, nc, stack):
        self.nc = nc
        self.stack = stack
        self.ops = {e: [] for e in ENGS}
        self.cs = {}
        for e in ("pe", "act", "dve", "pool"):
            sem = stack.enter_context(nc.semaphore("c_" + e))
            self.cs[e] = Stream(e, sem, "c")
        self.ds = []
        self.waited = {e: {} for e in ENGS}

    def dma_stream(self, name, step=16):
        sem = self.stack.enter_context(self.nc.semaphore("d_" + name))
        s = Stream(name, sem, "d", step)
        self.ds.append(s)
        return s

    def _wait(self, eng, stream, val):
        if val <= 0:
            return
        if self.waited[eng].get(stream.name, 0) >= val:
            return
        self.waited[eng][stream.name] = val
        sem = stream.sem
        self.ops[eng].append(lambda e, sem=sem, val=val: e.wait_ge(sem, val))

    def _deps(self, eng, reads, writes):
        mine = self.cs.get(eng)
        for r in reads:
            if r.w is not None:
                s, idx = r.w
                self._wait(eng, s, s.count * s.step if s.kind == "d" else idx + 1)
        for w in writes:
            if w.w is not None:
                s, idx = w.w
                if s is not mine:
                    self._wait(eng, s, s.count * s.step if s.kind == "d" else idx + 1)
            for s, idx in w.rs.values():
                if s is not mine:
                    self._wait(eng, s, s.count * s.step if s.kind == "d" else idx + 1)

    def _mark(self, tag, reads, writes, convert=True):
        s, idx = tag
        for r in reads:
            r.rs[s.name] = tag
            if convert and r.w is not None and r.w[0].kind == "d" and s.kind == "c":
                r.w = tag
        for w in writes:
            w.w = tag
            w.rs = {}

    def op(self, eng, fn, r=(), w=(), inc=True):
        self._deps(eng, r, w)
        st = self.cs[eng]
        idx = st.count
        if inc:
            st.count += 1
            sem = st.sem
            self.ops[eng].append(lambda e, fn=fn, sem=sem: fn(e).then_inc(sem, 1))
        else:
            self.ops[eng].append(lambda e, fn=fn: fn(e))
        self._mark((st, idx), r, w, convert=inc)

    def dma(self, eng, stream, out, in_, r=(), w=(), **kw):
        self._deps(eng, r, w)
        idx = stream.count
        stream.count += 1
        sem = stream.sem
        self.ops[eng].append(
            lambda e, out=out, in_=in_, sem=sem, kw=kw: e.dma_start(out=out, in_=in_, **kw).then_inc(sem, 16))
        self._mark((stream, idx), r, w)

    def custom(self, eng, stream, fn, r=(), w=()):
        self._deps(eng, r, w)
        idx = stream.count
        stream.count += 1
        sem = stream.sem
        step = stream.step
        self.ops[eng].append(lambda e, fn=fn, sem=sem, step=step: fn(e).then_inc(sem, step))
        self._mark((stream, idx), r, w)

    def barrier(self):
        for e in ENGS:
            for s in list(self.cs.values()) + self.ds:
                if s.kind == "d":
                    self._wait(e, s, s.count * s.step)
                else:
                    self._wait(e, s, s.count)

    def flush(self, block):
        decs = {"pe": block.tensor, "act": block.scalar, "dve": block.vector, "pool": block.gpsimd, "sp": block.sync}
        for e in ENGS:
            ops = self.ops[e]
            self.ops[e] = []
            if not ops:
                continue

            def body(eng, ops=ops):
                for o in ops:
                    o(eng)
            decs[e](body)


def f_act(out, in_, func, **kw):
    return lambda e: e.activation(out=out, in_=in_, func=func, **kw)


def f_ts(out, in0, s1, s2, op0, op1=None, **kw):
    if op1 is None:
        return lambda e: e.tensor_scalar(out=out, in0=in0, scalar1=s1, scalar2=s2, op0=op0, **kw)
    return lambda e: e.tensor_scalar(out=out, in0=in0, scalar1=s1, scalar2=s2, op0=op0, op1=op1, **kw)


def f_tt(out, in0, in1, op):
    return lambda e: e.tensor_tensor(out=out, in0=in0, in1=in1, op=op)


def f_stt(out, in0, scalar, in1, op0, op1, **kw):
    return lambda e: e.scalar_tensor_tensor(out=out, in0=in0, scalar=scalar, in1=in1, op0=op0, op1=op1, **kw)


def f_copy(out, in_):
    return lambda e: e.tensor_copy(out=out, in_=in_)


def f_memset(ap, v):
    return lambda e: e.memset(ap, v)


def f_mm(out, lhsT, rhs, start, stop):
    return lambda e: e.matmul(out, lhsT=lhsT, rhs=rhs, start=start, stop=stop)


def f_tr(out, in_, ident):
    return lambda e: e.transpose(out=out, in_=in_, identity=ident)


def f_red(out, in_, op, axis=AX.X):
    return lambda e: e.tensor_reduce(out=out, in_=in_, axis=axis, op=op)


def f_recip(out, in_):
    return lambda e: e.reciprocal(out=out, in_=in_)


class Ctx:
    def __init__(self, nc, st):
        self.nc, self.st = nc, st
        self.n = 0

    def sb(self, name, shape, dt):
        return self.st.enter_context(self.nc.sbuf_tensor(name, list(shape), dt))

    def ps(self, name):
        return self.st.enter_context(self.nc.psum_tensor(name, [128, 512], F32))


def phase1(nc, P, T, ph):
    with ExitStack() as st:
        C = Ctx(nc, st)
        W = C.sb(ph + "W", [128, 32, NCOL], BF16)
        wst = [C.sb(ph + f"wst{i}", [128, NCOL], F32) for i in range(2)]
        n1w = C.sb(ph + "n1w", [128, 32], F32)
        xt = [C.sb(ph + f"xt{i}", [128, D], F32) for i in range(2)]
        u = [C.sb(ph + f"u{i}", [128, D], BF16) for i in range(2)]
        uT = C.sb(ph + "uT", [128, 32, 512], BF16)
        stat = C.sb(ph + "stat", [128, 8], F32)
        idb = C.sb(ph + "idb", [128, 128], BF16)
        fmst = [C.sb(ph + f"fmst{i}", [128, 512], BF16) for i in range(2)]
        vst = [C.sb(ph + f"vst{i}", [128, 256], BF16) for i in range(2)]
        zst = [C.sb(ph + f"zst{i}", [128, 256], F32) for i in range(2)]
        dst = [C.sb(ph + f"dst{i}", [128, 4], F32) for i in range(2)]
        pT = [C.ps(ph + f"pT{i}") for i in range(2)]
        pF = [C.ps(ph + f"pF{i}") for i in range(2)]
        pA = [C.ps(ph + f"pA{i}") for i in range(2)]
        pB = [C.ps(ph + f"pB{i}") for i in range(2)]
        pTb = [p[:].bitcast(BF16) for p in pT]

        r_W = [Res() for _ in range(32)]
        r_wst = [Res(), Res()]
        r_n1w, r_idb = Res(), Res()
        r_xt = [Res(), Res()]
        r_u = [[Res(), Res()], [Res(), Res()]]
        r_ss = [Res(), Res()]
        r_rs = [Res(), Res()]
        r_uT = [[Res() for _ in range(4)] for _ in range(4)]
        r_pT = [Res(), Res()]
        r_pF = [Res(), Res()]
        r_pA = [Res(), Res()]
        r_pB = [Res(), Res()]
        r_fmst = [Res(), Res()]
        r_vst = [Res(), Res()]
        r_zst = [Res(), Res()]
        r_dst = [Res(), Res()]

        d_w = [P.dma_stream(ph + "w0"), P.dma_stream(ph + "w1")]
        d_x = [P.dma_stream(ph + "x0"), P.dma_stream(ph + "x1")]
        d_c = P.dma_stream(ph + "c")
        d_o = P.dma_stream(ph + "o")

        P.dma("sp", d_c, n1w[:], T["n1w"][:, :], w=[r_n1w])
        P.dma("sp", P.dma_stream(ph + "c2"), idb[:], T["idb"][:, :], w=[r_idb])
        for k in range(32):
            s = k % 2
            P.dma("sp", d_w[s], wst[s][:], T["w_in"][k * 128:(k + 1) * 128, :], w=[r_wst[s]])
            if k % 2 == 0:
                P.op("act", f_act(W[:, k, :], wst[s][:], AF.Copy, scale=n1w[:, k:k + 1]),
                     r=[r_wst[s], r_n1w], w=[r_W[k]])
            else:
                P.op("dve", f_ts(W[:, k, :], wst[s][:], n1w[:, k:k + 1], None, ALU.mult),
                     r=[r_wst[s], r_n1w], w=[r_W[k]])

        ngroups = (NT + 3) // 4
        ntr = 0
        for g in range(ngroups):
            nt = min(4, NT - 4 * g)
            ntok = nt * 128
            tok0 = g * 512
            for t in range(nt):
                f = g * 4 + t
                b, i = divmod(f, TPB)
                s = f % 2
                if i == 0:
                    P.op("pool", f_memset(xt[s][:], 0.0), w=[r_xt[s]])
                    P.dma("sp", d_x[s], xt[s][112:128, :], T["meta"][:, :], w=[r_xt[s]])
                else:
                    row = b * SEQ + (i - 1) * 128
                    P.dma("sp", d_x[s], xt[s][:], T["x"][row:row + 128, :], w=[r_xt[s]])
                P.op("act", f_act(u[s][:], xt[s][:], AF.Square, accum_out=stat[:, s:s + 1]),
                     r=[r_xt[s]], w=[r_u[s][0], r_u[s][1], r_ss[s]])
                P.op("act", f_act(stat[:, 2 + s:3 + s], stat[:, s:s + 1], AF.Sqrt, scale=1.0 / D, bias=EPS),
                     r=[r_ss[s]], w=[r_rs[s]])
                P.op("dve", f_recip(stat[:, 4 + s:5 + s], stat[:, 2 + s:3 + s]), r=[r_rs[s]], w=[r_rs[s]])
                rstd = stat[:, 4 + s:5 + s]
                P.op("act", f_act(u[s][:, 0:2048], xt[s][:, 0:2048], AF.Copy, scale=rstd),
                     r=[r_xt[s], r_rs[s]], w=[r_u[s][0]])
                P.op("dve", f_ts(u[s][:, 2048:4096], xt[s][:, 2048:4096], rstd, None, ALU.mult),
                     r=[r_xt[s], r_rs[s]], w=[r_u[s][1]])
                for kb in range(4):
                    pb = ntr % 2
                    ntr += 1
                    for kk in range(8):
                        k = kb * 8 + kk
                        P.op("pe", f_tr(pTb[pb][:, kk * 128:(kk + 1) * 128], u[s][:, k * 128:(k + 1) * 128], idb[:]),
                             r=[r_u[s][k // 16], r_idb], w=[r_pT[pb]], inc=(kk == 7))
                    dst_ap = uT[:, kb * 8:(kb + 1) * 8, t * 128:(t + 1) * 128]
                    src_ap = pTb[pb][:, :].rearrange("p (a b) -> p a b", a=8)
                    eng = "act" if kb % 2 == 0 else "dve"
                    if eng == "act":
                        P.op("act", f_act(dst_ap, src_ap, AF.Copy), r=[r_pT[pb]], w=[r_uT[t][kb]])
                    else:
                        P.op("dve", f_copy(dst_ap, src_ap), r=[r_pT[pb]], w=[r_uT[t][kb]])
            for j in range(8):
                pb = j % 2
                for k in range(32):
                    P.op("pe", f_mm(pF[pb][:, 0:ntok], W[:, k, j * 128:(j + 1) * 128], uT[:, k, 0:ntok], k == 0, k == 31),
                         r=[r_W[k]] + [r_uT[t][k // 8] for t in range(nt)], w=[r_pF[pb]], inc=(k == 31))
                if j % 2 == 0:
                    P.op("act", f_act(fmst[pb][:, 0:ntok], pF[pb][:, 0:ntok], AF.Copy), r=[r_pF[pb]], w=[r_fmst[pb]])
                else:
                    P.op("dve", f_copy(fmst[pb][:, 0:ntok], pF[pb][:, 0:ntok]), r=[r_pF[pb]], w=[r_fmst[pb]])
                P.dma("sp", d_o, T["fm_scr"][j, :, tok0:tok0 + ntok], fmst[pb][:, 0:ntok], r=[r_fmst[pb]])
            for t in range(nt):
                pb = t % 2
                row = tok0 + t * 128
                for k in range(32):
                    P.op("pe", f_mm(pA[pb][:, 0:260], uT[:, k, t * 128:(t + 1) * 128], W[:, k, 1024:1284], k == 0, k == 31),
                         r=[r_W[k], r_uT[t][k // 8]], w=[r_pA[pb]], inc=False)
                    P.op("pe", f_mm(pB[pb][:, 0:256], uT[:, k, t * 128:(t + 1) * 128], W[:, k, 1284:1540], k == 0, k == 31),
                         r=[r_W[k], r_uT[t][k // 8]], w=[r_pB[pb]], inc=(k == 31))
                P.op("act", f_act(vst[pb][:], pA[pb][:, 0:256], AF.Copy), r=[r_pA[pb]], w=[r_vst[pb]])
                P.op("dve", f_copy(dst[pb][:], pA[pb][:, 256:260]), r=[r_pA[pb]], w=[r_dst[pb]])
                P.op("dve", f_copy(zst[pb][:], pB[pb][:, 0:256]), r=[r_pB[pb]], w=[r_zst[pb]])
                P.dma("sp", d_o, T["v_scr"][row:row + 128, :], vst[pb][:], r=[r_vst[pb]])
                P.dma("sp", d_o, T["dt_scr"][row:row + 128, :], dst[pb][:], r=[r_dst[pb]])
                P.dma("sp", d_o, T["z_scr"][row:row + 128, :], zst[pb][:], r=[r_zst[pb]])
        P.barrier()
        with nc.Block() as block:
            P.flush(block)


PP = {}
_o = 0
for _n, _w in (("convw", 16), ("convb", 4), ("biasT", 256), ("maskT", 256), ("c31", 1), ("alog", 4), ("dtb", 4),
               ("dsk", 4), ("ssmw", 256), ("subw", 256), ("lq1", 128), ("lk1", 128), ("lq2", 128), ("lk2", 128),
               ("tri", 128), ("mneg", 128), ("padm", 1), ("ones", 128)):
    PP[_n] = (_o, _o + _w)
    _o += _w
NPP = _o


def phase2(nc, P, T, ph):
    with ExitStack() as st:
        C = Ctx(nc, st)
        pp = C.sb(ph + "pp", [128, NPP], F32)
        idb = C.sb(ph + "idb", [128, 128], BF16)
        KT = C.sb(ph + "KT", [128, 2, TPB * 128], BF16)
        V = C.sb(ph + "V", [128, TPB, 256], BF16)
        xin = C.sb(ph + "xin", [128, 4, 3 + TPB * 128], BF16)
        HW2 = TPB * 128 // 2
        acc = C.sb(ph + "acc", [128, 2, HW2], F32)
        S = [C.sb(ph + f"S{i}", [128, TPB * 128], F32) for i in range(2)]
        P0 = [C.sb(ph + f"P0{i}", [128, TPB * 128], BF16) for i in range(2)]
        P1 = C.sb(ph + "P1", [128, TPB * 128], BF16)
        aT = C.sb(ph + "aT", [128, TPB, 128], BF16)
        Qt = [C.sb(ph + f"Qt{i}", [128, 2, 128], BF16) for i in range(2)]
        dtr = C.sb(ph + "dtr", [128, TPB, 4], F32)
        dt = C.sb(ph + "dt", [128, TPB, 4], F32)
        adt = C.sb(ph + "adt", [128, TPB, 4], F32)
        sp1 = C.sb(ph + "sp1", [128, TPB, 4], F32)
        sp2 = C.sb(ph + "sp2", [128, TPB, 4], F32)
        sm = C.sb(ph + "sm", [128, 64], F32)
        a_b = C.sb(ph + "a_b", [128, 4], F32)
        biasm = C.sb(ph + "biasm", [128, 256], F32)
        subw8 = C.sb(ph + "subw8", [128, 256], F32)
        lamt = C.sb(ph + "lamt", [128, 128], F32)
        zt = [C.sb(ph + f"zt{i}", [128, 256], F32) for i in range(2)]
        zraw = [C.sb(ph + f"zraw{i}", [128, 256], F32) for i in range(2)]
        ez = C.sb(ph + "ez", [128, 256], F32)
        xs = C.sb(ph + "xs", [128, 256], F32)
        Btok = C.sb(ph + "Btok", [128, 128], BF16)
        Xb = C.sb(ph + "Xb", [128, 4, 64], BF16)
        Xd = C.sb(ph + "Xd", [128, 4, 64], BF16)
        R = C.sb(ph + "R", [128, 4, 128], F32)
        arg = C.sb(ph + "arg", [128, 4, 128], F32)
        dec = C.sb(ph + "dec", [128, 4, 128], F32)
        MT = C.sb(ph + "MT", [128, 4, 128], BF16)
        acs = C.sb(ph + "acs", [128, 16], F32)
        dta = C.sb(ph + "dta", [128, 4], F32)
        y = C.sb(ph + "y", [128, 4, 64], F32)
        ytmp = C.sb(ph + "ytmp", [128, 4, 64], F32)
        g = C.sb(ph + "g", [128, 256], F32)
        gsq = C.sb(ph + "gsq", [128, 256], F32)
        h = C.sb(ph + "h", [128, 4, 64], F32)
        hb = C.sb(ph + "hb", [128, 256], BF16)
        osb = C.sb(ph + "osb", [128, 256], F32)
        osq = C.sb(ph + "osq", [128, 256], F32)
        mixa = C.sb(ph + "mixa", [128, 256], BF16)
        mixs = C.sb(ph + "mixs", [128, 256], BF16)
        mta = [C.sb(ph + f"mta{i}", [128, 2, 128], BF16) for i in range(2)]
        mts = [C.sb(ph + f"mts{i}", [128, 2, 128], BF16) for i in range(2)]
        pS = [C.ps(ph + f"pS{i}") for i in range(2)]
        pT = C.ps(ph + "pT")
        pO = C.ps(ph + "pO")
        pX = C.ps(ph + "pX")
        pC = C.ps(ph + "pC")
        pM = C.ps(ph + "pM")
        pY = C.ps(ph + "pY")
        pTb = pT[:].bitcast(BF16)
        pOb = pO[:].bitcast(BF16)
        pXb = pX[:].bitcast(BF16)

        def col(n, a=None, b=None):
            lo, hi = PP[n]
            if a is None:
                return pp[:, lo:hi]
            return pp[:, lo + a:lo + b]

        r_pp, r_idb = Res(), Res()
        r_KT, r_V, r_xin, r_acc = Res(), Res(), [Res() for _ in range(4)], [Res(), Res()]
        r_S, r_P0, r_P1, r_aT = [Res(), Res()], [Res(), Res()], Res(), Res()
        r_Qt = [Res(), Res()]
        r_dt, r_sm, r_const = Res(), Res(), Res()
        r_zt = [Res(), Res()]
        r_zraw = [Res(), Res()]
        r_pS, r_pT, r_pO, r_pX, r_pC, r_pM, r_pY = [Res(), Res()], Res(), Res(), Res(), Res(), Res(), Res()
        r_xs, r_Btok, r_Xb, r_Xd, r_R, r_arg, r_dec, r_MT, r_acs, r_dta = (Res() for _ in range(10))
        r_y, r_ytmp, r_g, r_gsq, r_h, r_hb, r_osb, r_osq, r_mixa, r_mixs, r_ez = (Res() for _ in range(11))
        r_mta, r_mts = [Res(), Res()], [Res(), Res()]
        r_l = Res()

        d_c = P.dma_stream(ph + "c")
        d_b = P.dma_stream(ph + "b")
        d_b2 = P.dma_stream(ph + "b2")
        d_q = [P.dma_stream(ph + "q0"), P.dma_stream(ph + "q1")]
        d_z = [P.dma_stream(ph + "z0"), P.dma_stream(ph + "z1")]
        d_o = P.dma_stream(ph + "o")

        P.dma("sp", d_c, pp[:], T["pp"][:, :], w=[r_pp])
        P.dma("sp", P.dma_stream(ph + "c2"), idb[:], T["idb"][:, :], w=[r_idb])
        P.op("act", f_act(a_b[:], col("alog"), AF.Exp), r=[r_pp], w=[r_const])
        P.op("dve", f_ts(a_b[:], a_b[:], -1.0, None, ALU.mult), r=[r_const], w=[r_const])
        P.op("dve", f_stt(biasm[:], col("biasT"), col("c31"), col("maskT"), ALU.subtract, ALU.add), r=[r_pp], w=[r_const])
        P.op("dve", f_ts(subw8[:], col("subw"), 1.0 - LAMBDA_INIT, None, ALU.mult), r=[r_pp], w=[r_const])
        P.op("dve", f_tt(lamt[:], col("lq1"), col("lk1"), ALU.mult), r=[r_pp], w=[r_sm])
        P.op("dve", f_red(sm[:, 0:1], lamt[:], ALU.add), r=[r_sm], w=[r_sm])
        P.op("dve", f_tt(lamt[:], col("lq2"), col("lk2"), ALU.mult), r=[r_sm, r_pp], w=[r_sm])
        P.op("dve", f_red(sm[:, 1:2], lamt[:], ALU.add), r=[r_sm], w=[r_sm])
        P.op("act", f_act(sm[:, 2:4], sm[:, 0:2], AF.Exp), r=[r_sm], w=[r_sm])
        P.op("dve", f_tt(sm[:, 4:5], sm[:, 3:4], sm[:, 2:3], ALU.subtract), r=[r_sm], w=[r_sm])
        P.op("dve", f_ts(sm[:, 4:5], sm[:, 4:5], -LAMBDA_INIT, None, ALU.add), r=[r_sm], w=[r_const])
        neglam = sm[:, 4:5]
        nbat = 0 if 'stop_const' in DBG else NB

        for b in range(nbat):
            tb0 = b * TPB * 128
            for m in range(2):
                P.dma("sp", d_b, KT[:, m, :], T["fm_scr"][2 + m, :, tb0:tb0 + TPB * 128], w=[r_KT])
            P.dma("sp", d_b, V[:], T["v_scr"][tb0:tb0 + TPB * 128, :].rearrange("(i p) c -> p i c", p=128), w=[r_V])
            for j in range(4):
                P.op("pool", f_memset(xin[:, j, 0:3], 0.0), w=[r_xin[j]])
                P.dma("sp", d_b2, xin[:, j, 3:], T["fm_scr"][4 + j, :, tb0:tb0 + TPB * 128], w=[r_xin[j]])
            P.dma("sp", d_b2, dtr[:], T["dt_scr"][tb0:tb0 + TPB * 128, :].rearrange("(i p) c -> p i c", p=128), w=[r_dt])
            dtb_b = col("dtb").unsqueeze(1).to_broadcast([128, TPB, 4])
            P.op("dve", f_tt(dtr[:], dtr[:], dtb_b, ALU.add), r=[r_dt, r_pp], w=[r_dt])
            P.op("dve", f_ts(sp1[:], dtr[:], -1.0, None, ALU.mult), r=[r_dt], w=[r_dt])
            P.op("dve", f_tt(sp1[:], sp1[:], dtr[:], ALU.max), r=[r_dt], w=[r_dt])
            P.op("act", f_act(sp2[:], sp1[:], AF.Exp, scale=-1.0), r=[r_dt], w=[r_dt])
            P.op("act", f_act(sp2[:], sp2[:], AF.Ln, bias=1.0), r=[r_dt], w=[r_dt])
            P.op("dve", f_ts(sp1[:], dtr[:], 0.0, None, ALU.max), r=[r_dt], w=[r_dt])
            P.op("dve", f_tt(dt[:], sp1[:], sp2[:], ALU.add), r=[r_dt], w=[r_dt])
            P.op("dve", f_ts(dt[:, 0, :], dt[:, 0, :], col("padm"), None, ALU.mult), r=[r_dt, r_pp], w=[r_dt])
            P.op("dve", f_tt(adt[:], dt[:], a_b[:].unsqueeze(1).to_broadcast([128, TPB, 4]), ALU.mult),
                 r=[r_dt, r_const], w=[r_dt])
            for j in range(4):
                for hf in range(2):
                    c0 = hf * HW2
                    cw = lambda tap, j=j: col("convw", j * 4 + tap, j * 4 + tap + 1)
                    P.op("dve", f_ts(acc[:, hf, :], xin[:, j, c0 + 3:c0 + 3 + HW2], cw(3), col("convb", j, j + 1), ALU.mult, ALU.add),
                         r=[r_xin[j], r_pp], w=[r_acc[hf]])
                    for tap in (2, 1, 0):
                        P.op("dve", f_stt(acc[:, hf, :], xin[:, j, c0 + tap:c0 + tap + HW2], cw(tap), acc[:, hf, :], ALU.mult, ALU.add),
                             r=[r_xin[j], r_acc[hf], r_pp], w=[r_acc[hf]])
                for hf in range(2):
                    c0 = hf * HW2
                    P.op("act", f_act(xin[:, j, c0 + 3:c0 + 3 + HW2], acc[:, hf, :], AF.Silu), r=[r_acc[hf]], w=[r_xin[j]])
            for m in range(2):
                P.op("pool", f_memset(P0[m][:, 0:112], 0.0), w=[r_P0[m]])

            nq = 0
            for i in range(0 if 'stop_conv' in DBG else TPB):
                tk = slice(i * 128, (i + 1) * 128)
                zs = i % 2
                if i >= 1:
                    zrow = tb0 + i * 128
                    P.dma("sp", d_z[zs], zraw[zs][:], T["z_scr"][zrow:zrow + 128, :], w=[r_zraw[zs]])
                    P.op("act", f_act(zt[zs][:], zraw[zs][:], AF.Copy), r=[r_zraw[zs]], w=[r_zt[zs]])
                for jj in range(3):
                    P.op("pe", f_tr(pXb[:, jj * 128:(jj + 1) * 128], xin[:, jj, 3 + i * 128:3 + (i + 1) * 128], idb[:]),
                         r=[r_xin[jj], r_idb], w=[r_pX], inc=(jj == 2))
                P.op("act", f_act(xs[:], pXb[:, 0:256], AF.Copy), r=[r_pX], w=[r_xs])
                P.op("act", f_act(Btok[:], pXb[:, 256:384], AF.Copy), r=[r_pX], w=[r_Btok])
                dt_b = dt[:, i, :].unsqueeze(2).to_broadcast([128, 4, 64])
                P.op("dve", f_tt(Xb[:], pXb[:, 0:256].rearrange("p (r c) -> p r c", r=4), dt_b, ALU.mult),
                     r=[r_pX, r_dt], w=[r_Xb])
                if SSD_LIM >= 2:
                    P.op("pool", f_tt(R[:], col("tri").unsqueeze(1).to_broadcast([128, 4, 128]),
                                      adt[:, i, :].unsqueeze(2).to_broadcast([128, 4, 128]), ALU.mult),
                         r=[r_pp, r_dt], w=[r_R])
                    P.op("pe", f_mm(pC[:, 0:512], col("ones"), R[:].rearrange("p r l -> p (r l)"), True, True),
                         r=[r_pp, r_R], w=[r_pC])
                    P.op("pe", f_mm(pM[:, 0:4], col("tri"), adt[:, i, :], True, True), r=[r_pp, r_dt], w=[r_pM])
                    P.op("dve", f_copy(acs[:, 0:4], pM[:, 0:4]), r=[r_pM], w=[r_acs])
                    pC3 = pC[:, 0:512].rearrange("p (r l) -> p r l", r=4)
                if SSD_LIM >= 3:
                    P.op("dve", f_tt(arg[:], pC3, acs[:, 0:4].unsqueeze(2).to_broadcast([128, 4, 128]), ALU.subtract),
                         r=[r_pC, r_acs], w=[r_arg])
                    P.op("dve", f_tt(arg[:], arg[:], col("mneg").unsqueeze(1).to_broadcast([128, 4, 128]), ALU.add),
                         r=[r_arg, r_pp], w=[r_arg])
                    P.op("act", f_act(dec[:], arg[:], AF.Exp), r=[r_arg], w=[r_dec])
                    P.op("act", f_act(acs[:, 4:8], acs[:, 0:4], AF.Exp), r=[r_acs], w=[r_acs])
                    P.op("dve", f_tt(dta[:], pC3[:, :, 127], acs[:, 0:4], ALU.subtract), r=[r_pC, r_acs], w=[r_dta])
                    P.op("act", f_act(acs[:, 8:12], dta[:], AF.Exp), r=[r_dta], w=[r_acs])
                    P.op("act", f_act(acs[:, 12:16], pC3[:, :, 127], AF.Exp), r=[r_pC], w=[r_acs])
                if SSD_LIM >= 4:
                    P.op("pe", f_mm(pM[:, 128:256], xin[:, 2, 3 + i * 128:3 + (i + 1) * 128],
                                    xin[:, 3, 3 + i * 128:3 + (i + 1) * 128], True, True),
                         r=[r_xin[2], r_xin[3]], w=[r_pM])
                if SSD_LIM >= 5:
                    if i >= 1:
                        P.op("dve", f_tt(MT[:], dec[:], pM[:, 128:256].unsqueeze(1).to_broadcast([128, 4, 128]), ALU.mult),
                             r=[r_dec, r_pM], w=[r_MT])
                        for rr in range(4):
                            P.op("pe", f_mm(pY[:, rr * 64:(rr + 1) * 64], MT[:, rr, :], Xb[:, rr, :], True, True),
                                 r=[r_MT, r_Xb], w=[r_pY], inc=False)
                        P.op("pe", f_mm(pY[:, 256:512], xin[:, 3, 3 + i * 128:3 + (i + 1) * 128], hb[:], True, True),
                             r=[r_xin[3], r_hb], w=[r_pY])
                        eb = acs[:, 4:8].unsqueeze(2).to_broadcast([128, 4, 64])
                        P.op("dve", f_tt(y[:], pY[:, 256:512].rearrange("p (r c) -> p r c", r=4), eb, ALU.mult),
                             r=[r_pY, r_acs], w=[r_y])
                        P.op("dve", f_tt(y[:], y[:], pY[:, 0:256].rearrange("p (r c) -> p r c", r=4), ALU.add),
                             r=[r_y, r_pY], w=[r_y])
                        P.op("pool", f_tt(ytmp[:], xs[:].rearrange("p (r c) -> p r c", r=4),
                                          col("dsk").unsqueeze(2).to_broadcast([128, 4, 64]), ALU.mult),
                             r=[r_xs, r_pp], w=[r_ytmp])
                        P.op("dve", f_tt(y[:], y[:], ytmp[:], ALU.add), r=[r_y, r_ytmp], w=[r_y])
                if SSD_LIM >= 6:
                    P.op("dve", f_tt(Xd[:], Xb[:], acs[:, 8:12].unsqueeze(2).to_broadcast([128, 4, 64]), ALU.mult),
                         r=[r_Xb, r_acs], w=[r_Xd])
                    P.op("pe", f_mm(pM[:, 256:512], Btok[:], Xd[:].rearrange("p r c -> p (r c)"), True, True),
                         r=[r_Btok, r_Xd], w=[r_pM])
                    pM3 = pM[:, 256:512].rearrange("p (r c) -> p r c", r=4)
                    if i == 0:
                        P.op("dve", f_copy(h[:], pM3), r=[r_pM], w=[r_h])
                    else:
                        P.op("dve", f_tt(h[:], h[:], acs[:, 12:16].unsqueeze(2).to_broadcast([128, 4, 64]), ALU.mult),
                             r=[r_h, r_acs], w=[r_h])
                        P.op("dve", f_tt(h[:], h[:], pM3, ALU.add), r=[r_h, r_pM], w=[r_h])
                    P.op("act", f_act(hb[:], h[:].rearrange("p r c -> p (r c)"), AF.Copy), r=[r_h], w=[r_hb])
                if SSD_LIM >= 7:
                    if i >= 1:
                        if S7 >= 1:
                            P.op("act", f_act(ez[:], zt[zs][:], AF.Exp, scale=-1.0), r=[r_zt[zs]], w=[r_ez])
                        if S7 >= 2:
                            P.op("dve", f_ts(ez[:], ez[:], 1.0, None, ALU.add), r=[r_ez], w=[r_ez])
                        if S7 >= 3:
                            P.op("dve", f_recip(ez[:], ez[:]), r=[r_ez], w=[r_ez])
                        if S7 >= 4:
                            P.op("dve", f_tt(g[:], y[:].rearrange("p r c -> p (r c)"), zt[zs][:], ALU.mult), r=[r_y, r_zt[zs]], w=[r_g])
                        if S7 >= 5:
                            P.op("dve", f_tt(g[:], g[:], ez[:], ALU.mult), r=[r_g, r_ez], w=[r_g])
                        if S7 >= 6:
                            P.op("pool", f_tt(gsq[:], g[:], g[:], ALU.mult), r=[r_g], w=[r_gsq])
                        if S7 >= 7:
                            P.op("dve", f_red(sm[:, 16:17], gsq[:], ALU.add), r=[r_gsq], w=[r_sm])
                        if S7 >= 8:
                            P.op("dve", f_ts(sm[:, 16:17], sm[:, 16:17], 1.0 / 256, EPS, ALU.mult, ALU.add), r=[r_sm], w=[r_sm])
                            if "s7g" in DBG:
                                P.op("act", f_act(dta[:, 2:3], g[:, 0:1], AF.Copy), r=[r_g], w=[Res()])
                            elif "s7ez" in DBG:
                                P.op("act", f_act(dta[:, 2:3], ez[:, 0:1], AF.Copy), r=[r_ez], w=[Res()])
                            elif "s7dummy" in DBG:
                                P.op("act", f_act(dta[:, 2:3], col("c31"), AF.Copy), r=[r_pp], w=[Res()])
                                P.op("act", f_act(sm[:, 17:18], sm[:, 16:17], AF.Sqrt), r=[r_sm], w=[r_sm])
                            elif "s7nodep" in DBG:
                                P.op("act", f_act(dta[:, 1:2], col("c31"), AF.Copy), r=[r_pp], w=[r_dta])
                            elif "s7sep" in DBG:
                                P.op("dve", f_copy(dta[:, 0:1], sm[:, 16:17]), r=[r_sm], w=[r_dta])
                                P.op("act", f_act(dta[:, 1:2], dta[:, 0:1], AF.Sqrt), r=[r_dta], w=[r_dta])
                            elif "s7dve" in DBG:
                                P.op("dve", f_copy(sm[:, 17:18], sm[:, 16:17]), r=[r_sm], w=[r_sm])
                            elif "s7other" in DBG:
                                P.op("act", f_act(acs[:, 4:5], sm[:, 16:17], AF.Copy), r=[r_sm], w=[r_acs])
                            else:
                                P.op("act", f_act(sm[:, 17:18], sm[:, 16:17], AF.Copy if "s7copy" in DBG else AF.Sqrt), r=[r_sm], w=[r_sm])
                        if S7 >= 9:
                            P.op("dve", f_recip(sm[:, 18:19], sm[:, 17:18]), r=[r_sm], w=[r_sm])
                        if S7 >= 10:
                            P.op("dve", f_stt(mixs[:], g[:], sm[:, 18:19], col("ssmw"), ALU.mult, ALU.mult), r=[r_g, r_sm, r_pp], w=[r_mixs])
                        ms = i % 2
                        for jj in range(0 if 'nomixtr' in DBG else 2):
                            P.op("pe", f_tr(pXb[:, 512 + jj * 128:512 + (jj + 1) * 128], mixs[:, jj * 128:(jj + 1) * 128], idb[:]),
                                 r=[r_mixs, r_idb], w=[r_pX], inc=(jj == 1))
                        P.op("act", f_act(mts[ms][:], pXb[:, 512:768].rearrange("p (a c) -> p a c", a=2), AF.Copy),
                             r=[r_pX], w=[r_mts[ms]])
                        otok = b * SEQ + (i - 1) * 128
                        if 'nomixdma' not in DBG:
                            oj, oo = divmod(otok, 1024)
                            P.dma("sp", d_o, T["mixT"][oj * 512 + 256:oj * 512 + 512, oo:oo + 128].rearrange("(a p) t -> p a t", p=128),
                                  mts[ms][:], r=[r_mts[ms]], w=[T["_r_mix"]])

                if i == 0 or 'noattn' in DBG:
                    continue
                qs = i % 2
                qcol = tb0 + i * 128
                for m in range(2):
                    P.dma("sp", d_q[qs], Qt[qs][:, m, :], T["fm_scr"][m, :, qcol:qcol + 128], w=[r_Qt[qs]])
                kend = (i + 1) * 128
                pb = i % 2
                nev = 0
                for m in range(2):
                    for k0 in range(0, kend, 512):
                        wd = min(512, kend - k0)
                        sb_ = nq % 2
                        nq += 1
                        P.op("pe", f_mm(pS[sb_][:, 0:wd], Qt[qs][:, m, :], KT[:, m, k0:k0 + wd], True, True),
                             r=[r_Qt[qs], r_KT], w=[r_pS[sb_]])
                        if nev % 2 == 0:
                            P.op("act", f_act(S[m][:, k0:k0 + wd], pS[sb_][:, 0:wd], AF.Copy, scale=SCALE),
                                 r=[r_pS[sb_]], w=[r_S[m]])
                        else:
                            P.op("dve", f_ts(S[m][:, k0:k0 + wd], pS[sb_][:, 0:wd], SCALE, None, ALU.mult),
                                 r=[r_pS[sb_]], w=[r_S[m]])
                        nev += 1
                    P.op("dve", f_tt(S[m][:, kend - 256:kend], S[m][:, kend - 256:kend], biasm[:], ALU.add),
                         r=[r_S[m], r_const], w=[r_S[m]])
                    P.op("dve", f_red(sm[:, 20 + m:21 + m], S[m][:, 112:kend], ALU.max), r=[r_S[m]], w=[r_l])
                    P.op("dve", f_ts(sm[:, 22 + m:23 + m], sm[:, 20 + m:21 + m], -1.0, None, ALU.mult), r=[r_l], w=[r_l])
                    dstP = P0[pb] if m == 0 else P1
                    P.op("act", f_act(dstP[:, 112:kend], S[m][:, 112:kend], AF.Exp, bias=sm[:, 22 + m:23 + m],
                                      accum_out=sm[:, 24 + m:25 + m]),
                         r=[r_S[m], r_l], w=[(r_P0[pb] if m == 0 else r_P1), r_l])
                P.op("dve", f_recip(sm[:, 26:28], sm[:, 24:26]), r=[r_l], w=[r_l])
                P.op("dve", f_tt(sm[:, 28:29], sm[:, 24:25], sm[:, 27:28], ALU.mult), r=[r_l], w=[r_l])
                P.op("dve", f_tt(sm[:, 29:30], sm[:, 28:29], neglam, ALU.mult), r=[r_l, r_const], w=[r_l])
                P.op("dve", f_stt(P0[pb][:, 112:kend], P1[:, 112:kend], sm[:, 29:30], P0[pb][:, 112:kend], ALU.mult, ALU.add),
                     r=[r_P1, r_P0[pb], r_l], w=[r_P0[pb]])
                nkt = i + 1
                for kg in range(0, nkt, 8):
                    ng = min(8, nkt - kg)
                    for kk in range(ng):
                        kt = kg + kk
                        P.op("pe", f_tr(pTb[:, kk * 128:(kk + 1) * 128], P0[pb][:, kt * 128:(kt + 1) * 128], idb[:]),
                             r=[r_P0[pb], r_idb], w=[r_pT], inc=(kk == ng - 1))
                    src_ap = pTb[:, 0:ng * 128].rearrange("p (a c) -> p a c", a=ng)
                    if (kg // 8) % 2 == 0:
                        P.op("act", f_act(aT[:, kg:kg + ng, :], src_ap, AF.Copy), r=[r_pT], w=[r_aT])
                    else:
                        P.op("dve", f_copy(aT[:, kg:kg + ng, :], src_ap), r=[r_pT], w=[r_aT])
                for kt in range(nkt):
                    P.op("pe", f_mm(pO[:, 0:256], aT[:, kt, :], V[:, kt, :], kt == 0, kt == nkt - 1),
                         r=[r_aT, r_V], w=[r_pO], inc=(kt == nkt - 1))
                P.op("dve", f_ts(osb[:], pO[:, 0:256], sm[:, 26:27], None, ALU.mult), r=[r_pO, r_l], w=[r_osb])
                P.op("pool", f_tt(osq[:], osb[:], osb[:], ALU.mult), r=[r_osb], w=[r_osq])
                P.op("dve", f_red(sm[:, 32:33], osq[:], ALU.add), r=[r_osq], w=[r_sm])
                P.op("dve", f_ts(sm[:, 32:33], sm[:, 32:33], 1.0 / 256, EPS, ALU.mult, ALU.add), r=[r_sm], w=[r_sm])
                P.op("act", f_act(sm[:, 33:34], sm[:, 32:33], AF.Sqrt), r=[r_sm], w=[r_sm])
                P.op("dve", f_recip(sm[:, 34:35], sm[:, 33:34]), r=[r_sm], w=[r_sm])
                P.op("dve", f_stt(mixa[:], osb[:], sm[:, 34:35], subw8[:], ALU.mult, ALU.mult), r=[r_osb, r_sm, r_const], w=[r_mixa])
                ma = i % 2
                for jj in range(2):
                    P.op("pe", f_tr(pOb[:, 512 + jj * 128:512 + (jj + 1) * 128], mixa[:, jj * 128:(jj + 1) * 128], idb[:]),
                         r=[r_mixa, r_idb], w=[r_pO], inc=(jj == 1))
                P.op("act", f_act(mta[ma][:], pOb[:, 512:768].rearrange("p (a c) -> p a c", a=2), AF.Copy),
                     r=[r_pO], w=[r_mta[ma]])
                otok = b * SEQ + (i - 1) * 128
                oj, oo = divmod(otok, 1024)
                P.dma("sp", d_o, T["mixT"][oj * 512:oj * 512 + 256, oo:oo + 128].rearrange("(a p) t -> p a t", p=128), mta[ma][:],
                      r=[r_mta[ma]], w=[T["_r_mix"]])
        P.barrier()
        with nc.Block() as block:
            P.flush(block)


def phase3a(nc, P, T, ph):
    with ExitStack() as st:
        C = Ctx(nc, st)
        mT = C.sb(ph + "mT", [128, 32, 1024], BF16)
        Wb = [C.sb(ph + f"Wb{i}", [128, 32, 512], BF16) for i in range(2)]
        wst = [C.sb(ph + f"wst{i}", [128, 4, 512], F32) for i in range(2)]
        xr = [C.sb(ph + f"xr{i}", [128, 512], F32) for i in range(2)]
        hb_ = [C.sb(ph + f"hb{i}", [128, 512], F32) for i in range(2)]
        pO = [C.ps(ph + f"pO{i}") for i in range(4)]
        r_mT = [Res() for _ in range(32)]
        r_Wb = [[Res() for _ in range(8)] for _ in range(2)]
        r_wst, r_xr, r_hb = [Res(), Res()], [Res(), Res()], [Res(), Res()]
        r_pO = [Res() for _ in range(4)]
        d_m = P.dma_stream(ph + "m")
        d_w = [P.dma_stream(ph + "w0"), P.dma_stream(ph + "w1")]
        d_x = [P.dma_stream(ph + "x0"), P.dma_stream(ph + "x1")]
        d_o = P.dma_stream(ph + "o")
        for kc in range(32):
            P.dma("sp", d_m, mT[:, kc, :], T["mT_in"][kc * 128:(kc + 1) * 128, :], w=[r_mT[kc]])
        wv = T["w_out"].rearrange("(kc p) n -> p kc n", p=128)
        n = 0
        nw = 0
        for dg in range(8):
            ws = dg % 2
            for kq in range(8):
                ss = nw % 2
                nw += 1
                P.dma("sp", d_w[ss], wst[ss][:], wv[:, kq * 4:(kq + 1) * 4, dg * 512:(dg + 1) * 512], w=[r_wst[ss]])
                if ss == 0:
                    P.op("act", f_act(Wb[ws][:, kq * 4:(kq + 1) * 4, :], wst[ss][:], AF.Copy), r=[r_wst[ss]], w=[r_Wb[ws][kq]])
                else:
                    P.op("dve", f_copy(Wb[ws][:, kq * 4:(kq + 1) * 4, :], wst[ss][:]), r=[r_wst[ss]], w=[r_Wb[ws][kq]])
            for tt in range(8):
                pb = n % 4
                xs_ = n % 2
                n += 1
                P.dma("sp", d_x[xs_], xr[xs_][:], T["xres"][tt * 128:(tt + 1) * 128, dg * 512:(dg + 1) * 512], w=[r_xr[xs_]])
                for kc in range(32):
                    P.op("pe", f_mm(pO[pb][:, :], mT[:, kc, tt * 128:(tt + 1) * 128], Wb[ws][:, kc, :], kc == 0, kc == 31),
                         r=[r_mT[kc], r_Wb[ws][kc // 4]], w=[r_pO[pb]], inc=(kc == 31))
                P.op("dve", f_tt(hb_[xs_][:], pO[pb][:, :], xr[xs_][:], ALU.add), r=[r_pO[pb], r_xr[xs_]], w=[r_hb[xs_]])
                P.dma("sp", d_o, T["h2"][tt * 128:(tt + 1) * 128, dg * 512:(dg + 1) * 512], hb_[xs_][:], r=[r_hb[xs_]],
                      w=[T["_r_h2"]])
        P.barrier()
        with nc.Block() as block:
            P.flush(block)


def phase3b(nc, P, T, ph):
    with ExitStack() as st:
        C = Ctx(nc, st)
        ht = [C.sb(ph + f"ht{i}", [128, D], F32) for i in range(2)]
        u2f = C.sb(ph + "u2f", [128, D], F32)
        u2h = [C.sb(ph + f"u2h{i}", [128, D], BF16) for i in range(2)]
        u2l = C.sb(ph + "u2l", [128, D], BF16)
        n2w = C.sb(ph + "n2w", [128, D], F32)
        uT = C.sb(ph + "uT", [128, 2, 32, 128], BF16)
        Wrf = C.sb(ph + "Wrf", [128, 32, 72], F32)
        Wrh = C.sb(ph + "Wrh", [128, 32, 72], BF16)
        Wrl = C.sb(ph + "Wrl", [128, 32, 72], BF16)
        Wrt = C.sb(ph + "Wrt", [128, 32, 72], F32)
        rb = C.sb(ph + "rb", [128, 72], F32)
        io8 = C.sb(ph + "io8", [128, 8], F32)
        idb = C.sb(ph + "idb", [128, 128], BF16)
        lg = C.sb(ph + "lg", [128, 72], F32)
        rt = [C.sb(ph + f"rt{i}", [128, 8], F32) for i in range(2)]
        stat = C.sb(ph + "stat", [128, 8], F32)
        q = C.sb(ph + "q", [128, 64], F32)
        goh = C.sb(ph + "goh", [128, 8], F32)
        ge = C.sb(ph + "ge", [128, 8], F32)
        el3 = C.sb(ph + "el3", [128, 8, 8], F32)
        els = C.sb(ph + "els", [128, 8], F32)
        msk = C.sb(ph + "msk", [128, 8], F32)
        oh1 = C.sb(ph + "oh1", [128, 8], F32)
        oh2 = C.sb(ph + "oh2", [128, 8], F32)
        tmp8 = C.sb(ph + "tmp8", [128, 8], F32)
        pT = [C.ps(ph + f"pT{i}") for i in range(2)]
        pL = C.ps(ph + "pL")
        pTb = [p[:].bitcast(BF16) for p in pT]
        r_ht, r_u2h, r_rt = [Res(), Res()], [Res(), Res()], [Res(), Res()]
        r_u2f, r_u2l, r_st, r_pL, r_lg, r_q = Res(), Res(), Res(), Res(), Res(), Res()
        r_uT = [[Res() for _ in range(4)] for _ in range(2)]
        r_pT = [Res(), Res()]
        r_n2w, r_wr, r_rb, r_idb, r_io = Res(), Res(), Res(), Res(), Res()
        d_h = [P.dma_stream(ph + "h0"), P.dma_stream(ph + "h1")]
        d_o = P.dma_stream(ph + "o")
        P.dma("sp", P.dma_stream(ph + "c1"), n2w[:], T["n2w"][:, :], w=[r_n2w])
        P.dma("sp", P.dma_stream(ph + "c2"), Wrf[:], T["wr"].rearrange("(kc p) n -> p kc n", p=128), w=[r_wr])
        P.dma("sp", P.dma_stream(ph + "c3"), rb[:], T["rb"][:, :], w=[r_rb])
        P.dma("sp", P.dma_stream(ph + "c4"), idb[:], T["idb"][:, :], w=[r_idb])
        P.dma("sp", P.dma_stream(ph + "c5"), io8[:], T["io8"][:, :], w=[r_io])
        P.op("dve", f_copy(Wrh[:], Wrf[:]), r=[r_wr], w=[r_wr])
        P.op("dve", f_copy(Wrt[:], Wrh[:]), r=[r_wr], w=[r_wr])
        P.op("dve", f_tt(Wrt[:], Wrf[:], Wrt[:], ALU.subtract), r=[r_wr], w=[r_wr])
        P.op("dve", f_copy(Wrl[:], Wrt[:]), r=[r_wr], w=[r_wr])
        ntr = 0
        for tt in range(8):
            s = tt % 2
            P.dma("sp", d_h[s], ht[s][:], T["h2"][tt * 128:(tt + 1) * 128, :], r=[T["_r_h2"]], w=[r_ht[s]])
            P.op("act", f_act(u2f[:], ht[s][:], AF.Square, accum_out=stat[:, 0:1]), r=[r_ht[s]], w=[r_u2f, r_st])
            P.op("act", f_act(stat[:, 1:2], stat[:, 0:1], AF.Sqrt, scale=1.0 / D, bias=EPS), r=[r_st], w=[r_st])
            P.op("dve", f_recip(stat[:, 2:3], stat[:, 1:2]), r=[r_st], w=[r_st])
            P.op("dve", f_stt(u2f[:], ht[s][:], stat[:, 2:3], n2w[:], ALU.mult, ALU.mult), r=[r_ht[s], r_st, r_n2w], w=[r_u2f])
            P.op("act", f_act(u2h[s][:], u2f[:], AF.Copy), r=[r_u2f], w=[r_u2h[s]])
            P.dma("sp", d_o, T["u2"][tt * 128:(tt + 1) * 128, :], u2h[s][:], r=[r_u2h[s]])
            P.op("dve", f_tt(u2f[:], u2f[:], u2h[s][:], ALU.subtract), r=[r_u2f, r_u2h[s]], w=[r_u2f])
            P.op("act", f_act(u2l[:], u2f[:], AF.Copy), r=[r_u2f], w=[r_u2l])
            for part, src, r_src in ((0, u2h[s], r_u2h[s]), (1, u2l, r_u2l)):
                for kb in range(4):
                    pb = ntr % 2
                    ntr += 1
                    for kk in range(8):
                        k = kb * 8 + kk
                        P.op("pe", f_tr(pTb[pb][:, kk * 128:(kk + 1) * 128], src[:, k * 128:(k + 1) * 128], idb[:]),
                             r=[r_src, r_idb], w=[r_pT[pb]], inc=(kk == 7))
                    dst_ap = uT[:, part, kb * 8:(kb + 1) * 8, :]
                    src_ap = pTb[pb][:, :].rearrange("p (a b) -> p a b", a=8)
                    if kb % 2 == 0:
                        P.op("act", f_act(dst_ap, src_ap, AF.Copy), r=[r_pT[pb]], w=[r_uT[part][kb]])
                    else:
                        P.op("dve", f_copy(dst_ap, src_ap), r=[r_pT[pb]], w=[r_uT[part][kb]])
            combos = [(0, Wrh), (1, Wrh), (0, Wrl)]
            nmm = 0
            for part, Wm in combos:
                for k in range(32):
                    P.op("pe", f_mm(pL[:, 0:72], uT[:, part, k, :], Wm[:, k, :], nmm == 0, nmm == 95),
                         r=[r_uT[part][k // 8], r_wr], w=[r_pL], inc=(nmm == 95))
                    nmm += 1
            P.op("dve", f_tt(lg[:], pL[:, 0:72], rb[:], ALU.add), r=[r_pL, r_rb], w=[r_lg])
            gl = lg[:, 0:8]
            P.op("dve", f_red(q[:, 0:1], gl, ALU.max), r=[r_lg], w=[r_q])
            P.op("dve", f_ts(goh[:], gl, q[:, 0:1], None, ALU.is_equal), r=[r_lg, r_q], w=[r_q])
            P.op("dve", f_ts(q[:, 1:2], q[:, 0:1], -1.0, None, ALU.mult), r=[r_q], w=[r_q])
            P.op("act", f_act(ge[:], gl, AF.Exp, bias=q[:, 1:2], accum_out=q[:, 2:3]), r=[r_lg, r_q], w=[r_q])
            P.op("dve", f_recip(q[:, 3:4], q[:, 2:3]), r=[r_q], w=[r_q])
            P.op("dve", f_tt(el3[:], lg[:, 8:72].rearrange("p (g e) -> p g e", g=8),
                             goh[:].unsqueeze(2).to_broadcast([128, 8, 8]), ALU.mult), r=[r_lg, r_q], w=[r_q])
            P.op("dve", f_red(els[:], el3[:].rearrange("p g e -> p e g"), ALU.add), r=[r_q], w=[r_q])
            P.op("dve", f_red(q[:, 4:5], els[:], ALU.max), r=[r_q], w=[r_q])
            P.op("dve", f_ts(oh1[:], els[:], q[:, 4:5], None, ALU.is_equal), r=[r_q], w=[r_q])
            P.op("dve", f_stt(msk[:], oh1[:], NEG, els[:], ALU.mult, ALU.add), r=[r_q], w=[r_q])
            P.op("dve", f_red(q[:, 5:6], msk[:], ALU.max), r=[r_q], w=[r_q])
            P.op("dve", f_ts(oh2[:], msk[:], q[:, 5:6], None, ALU.is_equal), r=[r_q], w=[r_q])
            P.op("dve", f_tt(q[:, 6:7], q[:, 5:6], q[:, 4:5], ALU.subtract), r=[r_q], w=[r_q])
            P.op("act", f_act(q[:, 7:8], q[:, 6:7], AF.Exp), r=[r_q], w=[r_q])
            P.op("dve", f_ts(q[:, 8:9], q[:, 7:8], 1.0, None, ALU.add), r=[r_q], w=[r_q])
            P.op("dve", f_recip(q[:, 9:10], q[:, 8:9]), r=[r_q], w=[r_q])
            P.op("dve", f_tt(rt[s][:, 2:3], q[:, 3:4], q[:, 9:10], ALU.mult), r=[r_q], w=[r_rt[s]])
            P.op("dve", f_tt(rt[s][:, 3:4], rt[s][:, 2:3], q[:, 7:8], ALU.mult), r=[r_q, r_rt[s]], w=[r_rt[s]])
            P.op("dve", f_tt(tmp8[:], goh[:], io8[:], ALU.mult), r=[r_q, r_io], w=[r_q])
            P.op("dve", f_red(q[:, 10:11], tmp8[:], ALU.add), r=[r_q], w=[r_q])
            P.op("dve", f_tt(tmp8[:], oh1[:], io8[:], ALU.mult), r=[r_q, r_io], w=[r_q])
            P.op("dve", f_red(q[:, 11:12], tmp8[:], ALU.add), r=[r_q], w=[r_q])
            P.op("dve", f_tt(tmp8[:], oh2[:], io8[:], ALU.mult), r=[r_q, r_io], w=[r_q])
            P.op("dve", f_red(q[:, 12:13], tmp8[:], ALU.add), r=[r_q], w=[r_q])
            P.op("dve", f_stt(rt[s][:, 0:1], q[:, 10:11], 8.0, q[:, 11:12], ALU.mult, ALU.add), r=[r_q, r_rt[s]], w=[r_rt[s]])
            P.op("dve", f_stt(rt[s][:, 1:2], q[:, 10:11], 8.0, q[:, 12:13], ALU.mult, ALU.add), r=[r_q, r_rt[s]], w=[r_rt[s]])
            P.op("dve", f_copy(rt[s][:, 4:8], lg[:, 0:4]), r=[r_lg, r_rt[s]], w=[r_rt[s]])
            P.dma("sp", d_o, T["route"][tt * 128:(tt + 1) * 128, :], rt[s][:], r=[r_rt[s]])
        P.barrier()
        with nc.Block() as block:
            P.flush(block)


def build_L2():
    nc = bass.Bass("TRN2", target_bir_lowering=False)
    T = {}
    for name, shape, dt in (("mT_in", [D, 1024], BF16), ("w_out", [D, D], F32), ("xres", [1024, D], F32),
                            ("n2w", [128, D], F32), ("wr", [D, 72], F32), ("rb", [128, 72], F32),
                            ("idb", [128, 128], BF16), ("io8", [128, 8], F32)):
        T[name] = nc.dram_tensor(name, list(shape), dt, kind="ExternalInput").ap()
    for name, shape, dt in (("h2", [1024, D], F32), ("u2", [1024, D], BF16), ("route", [1024, 8], F32)):
        T[name] = nc.dram_tensor(name, list(shape), dt, kind="ExternalOutput").ap()
    T["_r_h2"] = Res()
    with ExitStack() as stack:
        P = Prog(nc, stack)
        phase3a(nc, P, T, "a")
        phase3b(nc, P, T, "b")
    return nc


def build_L3(CAP):
    CH = min(CAP, 384)
    assert CAP % CH == 0 and CH % 128 == 0
    nch = CAP // CH
    nst = CH // 128
    nc = bass.Bass("TRN2", target_bir_lowering=False)
    T = {}
    for name, shape, dt in (("wg", [8, D, 768], F32), ("wu", [8, D, 768], F32), ("wd", [8, 768, D], F32),
                            ("xgT", [8, D, CAP], BF16), ("wts", [128, 8 * (CAP // 128)], F32)):
        T[name] = nc.dram_tensor(name, list(shape), dt, kind="ExternalInput").ap()
    T["y"] = nc.dram_tensor("y", [8, CAP, D], BF16, kind="ExternalOutput").ap()
    with ExitStack() as stack:
        P = Prog(nc, stack)
        C = Ctx(nc, stack)
        xT = C.sb("xT", [128, 32, CH], BF16)
        Wg = C.sb("Wg", [128, 32, 768], BF16)
        Wu = C.sb("Wu", [128, 32, 768], BF16)
        Wd = C.sb("Wd", [128, 6, D], BF16)
        gst = [C.sb(f"gst{i}", [128, 768], F32) for i in range(2)]
        ust = [C.sb(f"ust{i}", [128, 768], F32) for i in range(2)]
        dstg = [C.sb(f"dstg{i}", [128, 1024], F32) for i in range(2)]
        hT = C.sb("hT", [128, 6, CH], BF16)
        tmp = [C.sb(f"tmp{i}", [128, CH], F32) for i in range(2)]
        ys = [C.sb(f"ys{i}", [128, 512], BF16) for i in range(2)]
        wts = C.sb("wts_sb", [128, 8 * (CAP // 128)], F32)
        pG = [C.ps(f"pG{i}") for i in range(3)]
        pU = [C.ps(f"pU{i}") for i in range(3)]
        pD = [C.ps(f"pD{i}") for i in range(2)]
        r_xT, r_hT, r_wts = Res(), [Res() for _ in range(6)], Res()
        r_Wg, r_Wu = [Res() for _ in range(32)], [Res() for _ in range(32)]
        r_Wd = [[Res() for _ in range(4)] for _ in range(6)]
        r_gst, r_ust, r_dstg = [Res(), Res()], [Res(), Res()], [Res(), Res()]
        r_tmp, r_ys = [Res(), Res()], [Res(), Res()]
        r_pG, r_pU, r_pD = [Res() for _ in range(3)], [Res() for _ in range(3)], [Res(), Res()]
        d_x = P.dma_stream("x")
        d_g = [P.dma_stream("g0"), P.dma_stream("g1")]
        d_u = [P.dma_stream("u0"), P.dma_stream("u1")]
        d_d = [P.dma_stream("dd0"), P.dma_stream("dd1")]
        d_o = P.dma_stream("o")
        P.dma("sp", P.dma_stream("c"), wts[:], T["wts"][:, :], w=[r_wts])
        nd = 0
        ny = 0
        nt_ = 0
        for e in range(8):
            for k in range(32):
                s = k % 2
                P.dma("sp", d_g[s], gst[s][:], T["wg"][e, k * 128:(k + 1) * 128, :], w=[r_gst[s]])
                P.op("act", f_act(Wg[:, k, :], gst[s][:], AF.Copy), r=[r_gst[s]], w=[r_Wg[k]])
                P.dma("sp", d_u[s], ust[s][:], T["wu"][e, k * 128:(k + 1) * 128, :], w=[r_ust[s]])
                P.op("dve", f_copy(Wu[:, k, :], ust[s][:]), r=[r_ust[s]], w=[r_Wu[k]])
            for fc in range(6):
                for qd in range(4):
                    s = nd % 2
                    nd += 1
                    P.dma("sp", d_d[s], dstg[s][:], T["wd"][e, fc * 128:(fc + 1) * 128, qd * 1024:(qd + 1) * 1024], w=[r_dstg[s]])
                    if s == 0:
                        P.op("act", f_act(Wd[:, fc, qd * 1024:(qd + 1) * 1024], dstg[s][:], AF.Copy), r=[r_dstg[s]], w=[r_Wd[fc][qd]])
                    else:
                        P.op("dve", f_copy(Wd[:, fc, qd * 1024:(qd + 1) * 1024], dstg[s][:]), r=[r_dstg[s]], w=[r_Wd[fc][qd]])
            for ch in range(nch):
                c0 = ch * CH
                P.dma("sp", d_x, xT[:], T["xgT"][e, :, c0:c0 + CH].rearrange("(kc p) s -> p kc s", p=128), w=[r_xT])
                for fh in range(2):
                    for j in range(3):
                        ft = fh * 3 + j
                        for k in range(32):
                            P.op("pe", f_mm(pG[j][:, 0:CH], Wg[:, k, ft * 128:(ft + 1) * 128], xT[:, k, :], k == 0, k == 31),
                                 r=[r_Wg[k], r_xT], w=[r_pG[j]], inc=(k == 31))
                        for k in range(32):
                            P.op("pe", f_mm(pU[j][:, 0:CH], Wu[:, k, ft * 128:(ft + 1) * 128], xT[:, k, :], k == 0, k == 31),
                                 r=[r_Wu[k], r_xT], w=[r_pU[j]], inc=(k == 31))
                        ts_ = nt_ % 2
                        nt_ += 1
                        P.op("act", f_act(tmp[ts_][:], pG[j][:, 0:CH], AF.Silu), r=[r_pG[j]], w=[r_tmp[ts_]])
                        P.op("dve", f_tt(hT[:, ft, :], tmp[ts_][:], pU[j][:, 0:CH], ALU.mult), r=[r_tmp[ts_], r_pU[j]], w=[r_hT[ft]])
                for st_ in range(nst):
                    for dg in range(8):
                        pb = ny % 2
                        ny += 1
                        for fc in range(6):
                            P.op("pe", f_mm(pD[pb][:, :], hT[:, fc, st_ * 128:(st_ + 1) * 128], Wd[:, fc, dg * 512:(dg + 1) * 512],
                                            fc == 0, fc == 5),
                                 r=[r_hT[fc], r_Wd[fc][dg // 2]], w=[r_pD[pb]], inc=(fc == 5))
                        wcol = e * (CAP // 128) + ch * nst + st_
                        if pb == 0:
                            P.op("act", f_act(ys[pb][:], pD[pb][:, :], AF.Copy, scale=wts[:, wcol:wcol + 1]),
                                 r=[r_pD[pb], r_wts], w=[r_ys[pb]])
                        else:
                            P.op("dve", f_ts(ys[pb][:], pD[pb][:, :], wts[:, wcol:wcol + 1], None, ALU.mult),
                                 r=[r_pD[pb], r_wts], w=[r_ys[pb]])
                        row = c0 + st_ * 128
                        P.dma("sp", d_o, T["y"][e, row:row + 128, dg * 512:(dg + 1) * 512], ys[pb][:], r=[r_ys[pb]])
        P.barrier()
        with nc.Block() as block:
            P.flush(block)
    return nc


def build_L4():
    nc = bass.Bass("TRN2", target_bir_lowering=False)
    T = {}
    for name, shape, dt in (("h2", [1024, D], F32), ("ya", [1024, D], BF16), ("yb", [1024, D], BF16), ("fnw", [128, D], F32)):
        T[name] = nc.dram_tensor(name, list(shape), dt, kind="ExternalInput").ap()
    T["out"] = nc.dram_tensor("out", [1024, D], F32, kind="ExternalOutput").ap()
    with ExitStack() as stack:
        P = Prog(nc, stack)
        C = Ctx(nc, stack)
        ht = [C.sb(f"ht{i}", [128, D], F32) for i in range(2)]
        ya = [C.sb(f"ya{i}", [128, D], BF16) for i in range(2)]
        yb = [C.sb(f"yb{i}", [128, D], BF16) for i in range(2)]
        t3 = C.sb("t3", [128, D], F32)
        jk = C.sb("jk", [128, D], F32)
        ot = [C.sb(f"ot{i}", [128, D], F32) for i in range(2)]
        fnw = C.sb("fnw_sb", [128, D], F32)
        stat = C.sb("stat", [128, 8], F32)
        r_ht, r_ya, r_yb, r_ot = [Res(), Res()], [Res(), Res()], [Res(), Res()], [Res(), Res()]
        r_t3, r_jk, r_st, r_fnw = Res(), Res(), Res(), Res()
        d_h = [P.dma_stream("h0"), P.dma_stream("h1")]
        d_a = [P.dma_stream("a0"), P.dma_stream("a1")]
        d_b = [P.dma_stream("b0"), P.dma_stream("b1")]
        d_o = P.dma_stream("o")
        P.dma("sp", P.dma_stream("c"), fnw[:], T["fnw"][:, :], w=[r_fnw])
        for tt in range(8):
            s = tt % 2
            rows = slice(tt * 128, (tt + 1) * 128)
            P.dma("sp", d_h[s], ht[s][:], T["h2"][rows, :], w=[r_ht[s]])
            P.dma("sp", d_a[s], ya[s][:], T["ya"][rows, :], w=[r_ya[s]])
            P.dma("sp", d_b[s], yb[s][:], T["yb"][rows, :], w=[r_yb[s]])
            P.op("dve", f_tt(t3[:], ya[s][:], yb[s][:], ALU.add), r=[r_ya[s], r_yb[s]], w=[r_t3])
            P.op("dve", f_tt(t3[:], t3[:], ht[s][:], ALU.add), r=[r_t3, r_ht[s]], w=[r_t3])
            P.op("act", f_act(jk[:], t3[:], AF.Square, accum_out=stat[:, 0:1]), r=[r_t3], w=[r_jk, r_st])
            P.op("act", f_act(stat[:, 1:2], stat[:, 0:1], AF.Sqrt, scale=1.0 / D, bias=EPS), r=[r_st], w=[r_st])
            P.op("dve", f_recip(stat[:, 2:3], stat[:, 1:2]), r=[r_st], w=[r_st])
            P.op("dve", f_stt(ot[s][:], t3[:], stat[:, 2:3], fnw[:], ALU.mult, ALU.mult), r=[r_t3, r_st, r_fnw], w=[r_ot[s]])
            P.dma("sp", d_o, T["out"][rows, :], ot[s][:], r=[r_ot[s]])
        P.barrier()
        with nc.Block() as block:
            P.flush(block)
    return nc


def build_L1():
    nc = bass.Bass("TRN2", target_bir_lowering=False)
    T = {}
    for name, shape, dt in (("x", [NB * SEQ, D], F32), ("meta", [16, D], F32), ("w_in", [D, NCOL], F32),
                            ("n1w", [128, 32], F32), ("idb", [128, 128], BF16), ("pp", [128, NPP], F32)):
        T[name] = nc.dram_tensor(name, list(shape), dt, kind="ExternalInput").ap()
    for name, shape, dt in (("fm_scr", [8, 128, NTOK], BF16), ("v_scr", [NTOK, 256], BF16), ("z_scr", [NTOK, 256], F32),
                            ("dt_scr", [NTOK, 4], F32)):
        T[name] = nc.dram_tensor(name, list(shape), dt).ap()
    T["mixT"] = nc.dram_tensor("mixT", [(NB * SEQ // 1024) * 512, 1024], BF16, kind="ExternalOutput").ap()
    T["_r_mix"] = Res()
    with ExitStack() as stack:
        P = Prog(nc, stack)
        phase1(nc, P, T, "p1")
        phase2(nc, P, T, "p2")
    return nc


OFF_K, OFF_V, OFF_Z, OFF_X, OFF_B, OFF_C, OFF_DT = 2048, 4096, 6144, 8192, 10240, 11264, 12288


def core_cols(c):
    cols = []
    cols += list(range(c * 256, c * 256 + 256))
    cols += list(range(OFF_K + c * 256, OFF_K + c * 256 + 256))
    cols += list(range(OFF_X + c * 256, OFF_X + c * 256 + 256))
    cols += list(range(OFF_B + c * 128, OFF_B + c * 128 + 128))
    cols += list(range(OFF_C + c * 128, OFF_C + c * 128 + 128))
    cols += list(range(OFF_V + c * 256, OFF_V + c * 256 + 256))
    cols += list(range(OFF_DT + c * 4, OFF_DT + c * 4 + 4))
    cols += list(range(OFF_Z + c * 256, OFF_Z + c * 256 + 256))
    return np.array(cols)


def t5_bucket_np(rel):
    n = np.maximum(rel, 0)
    nf = np.maximum(n, 1).astype(np.float32)
    large = 16 + (np.log(nf / np.float32(16)) / np.float32(np.log(128 / 16)) * np.float32(16)).astype(np.int32)
    large = np.minimum(large, 31)
    return np.where(n < 16, n, large)


def make_pp(inp, c):
    pp = np.zeros((128, NPP), np.float32)

    def put(name, arr):
        lo, hi = PP[name]
        pp[:, lo:hi] = arr

    p = np.arange(128)
    chs = [c * 256 + p, c * 256 + 128 + p, 2048 + c * 128 + p, 3072 + c * 128 + p]
    cw = inp["conv_w"][0]
    cb = inp["conv_b"][0]
    convw = np.zeros((128, 16), np.float32)
    convb = np.zeros((128, 4), np.float32)
    for j in range(4):
        for tap in range(4):
            convw[:, j * 4 + tap] = cw[tap, chs[j]]
        convb[:, j] = cb[chs[j]]
    put("convw", convw)
    put("convb", convb)
    q = np.arange(128)[:, None]
    j = np.arange(256)[None, :]
    rel = q + 128 - j
    rb = inp["rel_bias"][:, c]
    put("biasT", np.where(rel >= 0, rb[t5_bucket_np(rel)], np.float32(0)))
    put("maskT", np.where(rel >= 0, np.float32(0), np.float32(NEG)))
    put("c31", np.full((128, 1), rb[31], np.float32))
    put("alog", np.tile(inp["a_log"][0][4 * c:4 * c + 4][None], (128, 1)))
    put("dtb", np.tile(inp["dt_bias"][0][4 * c:4 * c + 4][None], (128, 1)))
    put("dsk", np.tile(inp["d_skip"][0][4 * c:4 * c + 4][None], (128, 1)))
    put("ssmw", np.tile(inp["ssm_norm_w"][0][c * 256:(c + 1) * 256][None], (128, 1)))
    put("subw", np.tile(inp["subln_w"][0][None], (128, 1)))
    for n, k in (("lq1", "lambda_q1"), ("lk1", "lambda_k1"), ("lq2", "lambda_q2"), ("lk2", "lambda_k2")):
        put(n, np.tile(inp[k][0][None], (128, 1)))
    s_ = np.arange(128)[:, None]
    l_ = np.arange(128)[None, :]
    put("tri", (s_ <= l_).astype(np.float32))
    put("mneg", np.where(l_ >= s_, np.float32(0), np.float32(NEG)))
    put("padm", (np.arange(128) >= 112).astype(np.float32)[:, None])
    put("ones", np.ones((128, 128), np.float32))
    return pp


def host_consts(inp):
    rows = []
    for r in range(8):
        rows += list(range(r * 256, r * 256 + 256)) + list(range(2048 + r * 256, 2048 + r * 256 + 256))
    return {
        "w_out_p": np.ascontiguousarray(inp["w_out"][0][np.array(rows)]),
        "n2w": np.ascontiguousarray(np.tile(inp["norm2_w"][0][None], (128, 1))),
        "wr": np.ascontiguousarray(np.concatenate([inp["router_group_w"][0], inp["router_expert_w"][0]], axis=1)),
        "rb": np.ascontiguousarray(np.tile(np.concatenate([inp["router_group_b"][0], inp["router_expert_b"][0]])[None], (128, 1))),
        "idb": np.eye(128, dtype=np.float32).astype(ml_dtypes.bfloat16),
        "io8": np.ascontiguousarray(np.tile(np.arange(8, dtype=np.float32)[None], (128, 1))),
    }


def make_inputs(inp, cores):
    x = np.ascontiguousarray(inp["x"].reshape(NB * SEQ, D))
    meta = np.ascontiguousarray(inp["meta_tokens"])
    n1w = np.ascontiguousarray(inp["norm1_w"][0].reshape(32, 128).T)
    idb = np.eye(128, dtype=np.float32).astype(ml_dtypes.bfloat16)
    maps = []
    for c in cores:
        m = {"x": x, "meta": meta, "n1w": n1w, "idb": idb,
             "w_in": np.ascontiguousarray(inp["w_in"][0][:, core_cols(c)]),
             "pp": make_pp(inp, c)}
        maps.append(m)
    return maps


def kernel(**inp):
    cores = list(range(NCORES))
    x2 = np.ascontiguousarray(inp["x"].reshape(NB * SEQ, D))
    res1 = run_bass_kernel_spmd(build_L1(), make_inputs(inp, cores), core_ids=cores)
    mixT = [np.asarray(r["mixT"]) for r in res1.results]
    hc = host_consts(inp)
    maps2 = []
    for j in cores:
        mT = np.ascontiguousarray(np.concatenate([mixT[r][j * 512:(j + 1) * 512] for r in cores], axis=0))
        maps2.append({"mT_in": mT, "w_out": hc["w_out_p"], "xres": np.ascontiguousarray(x2[j * 1024:(j + 1) * 1024]),
                      "n2w": hc["n2w"], "wr": hc["wr"], "rb": hc["rb"], "idb": hc["idb"], "io8": hc["io8"]})
    res2 = run_bass_kernel_spmd(build_L2(), maps2, core_ids=cores)
    h2 = [np.asarray(r["h2"]) for r in res2.results]
    u2 = np.concatenate([np.asarray(r["u2"]) for r in res2.results], axis=0)
    route = np.concatenate([np.asarray(r["route"]) for r in res2.results], axis=0)
    ntok = NB * SEQ
    eid = route[:, 0:2].astype(np.int64)
    wgt = route[:, 2:4]
    lists = []
    for e in range(64):
        t0_ = np.nonzero(eid[:, 0] == e)[0]
        t1_ = np.nonzero(eid[:, 1] == e)[0]
        lists.append((t0_, t1_))
    mx = max(len(a_) + len(b_) for a_, b_ in lists)
    CAP = max(128, -(-mx // 128) * 128)
    if CAP > 384:
        CAP = -(-CAP // 384) * 384
    slot = np.zeros((ntok, 2), np.int64)
    maps3 = []
    for g in cores:
        xgT = np.zeros((8, D, CAP), ml_dtypes.bfloat16)
        wts = np.zeros((8, CAP), np.float32)
        for ee in range(8):
            t0_, t1_ = lists[g * 8 + ee]
            toks = np.concatenate([t0_, t1_])
            n_ = len(toks)
            xgT[ee, :, :n_] = u2[toks].T
            wts[ee, :len(t0_)] = wgt[t0_, 0]
            wts[ee, len(t0_):n_] = wgt[t1_, 1]
            slot[t0_, 0] = np.arange(len(t0_))
            slot[t1_, 1] = len(t0_) + np.arange(len(t1_))
        wts_t = np.ascontiguousarray(wts.reshape(8, CAP // 128, 128).transpose(2, 0, 1).reshape(128, 8 * (CAP // 128)))
        maps3.append({"wg": np.ascontiguousarray(inp["expert_w_gate"][0][g * 8:(g + 1) * 8]),
                      "wu": np.ascontiguousarray(inp["expert_w_up"][0][g * 8:(g + 1) * 8]),
                      "wd": np.ascontiguousarray(inp["expert_w_down"][0][g * 8:(g + 1) * 8]),
                      "xgT": xgT, "wts": wts_t})
    res3 = run_bass_kernel_spmd(build_L3(CAP), maps3, core_ids=cores)
    yall = np.stack([np.asarray(r["y"]) for r in res3.results], axis=0).reshape(64, CAP, D)
    ya = yall[eid[:, 0], slot[:, 0]]
    yb = yall[eid[:, 1], slot[:, 1]]
    fnw = np.ascontiguousarray(np.tile(inp["final_norm_w"][None], (128, 1)))
    maps4 = [{"h2": h2[j], "ya": np.ascontiguousarray(ya[j * 1024:(j + 1) * 1024]),
              "yb": np.ascontiguousarray(yb[j * 1024:(j + 1) * 1024]), "fnw": fnw} for j in cores]
    res4 = run_bass_kernel_spmd(build_L4(), maps4, core_ids=cores)
    out = np.concatenate([np.asarray(r["out"]) for r in res4.results], axis=0)
    return out.reshape(NB, SEQ, D).astype(np.float32)
```

```python
import numpy as np
import ml_dtypes
from contextlib import ExitStack

import concourse.bass as bass
import concourse.mybir as mybir
from concourse.bass_utils import run_bass_kernel_spmd

F32 = mybir.dt.float32
BF16 = mybir.dt.bfloat16
I32 = mybir.dt.int32
AF = mybir.ActivationFunctionType
ALU = mybir.AluOpType
AX = mybir.AxisListType

NCORES = 8
D = 4096
SEQ = 4096
NB = 2
TPB = 33
NT = NB * TPB
NTOK = NT * 128
NCOL = 1540
EPS = 1e-6
NEG = -1e30
SCALE = 128 ** -0.5
LAMBDA_INIT = 0.2
DBG = set()
SSD_LIM = 99
S7 = 99


class Stream:
    def __init__(self, name, sem, kind, step=16):
        self.name, self.sem, self.kind, self.count, self.step = name, sem, kind, 0, step


class Res:
    __slots__ = ("name", "w", "rs")

    def __init__(self, name=""):
        self.name = name
        self.w = None
        self.rs = {}


ENGS = ("pe", "act", "dve", "pool", "sp")


class Prog:
    def __init__(self, nc, stack):
        self.nc = nc
        self.stack = stack
        self.ops = {e: [] for e in ENGS}
        self.cs = {}
        for e in ("pe", "act", "dve", "pool"):
            sem = stack.enter_context(nc.semaphore("c_" + e))
            self.cs[e] = Stream(e, sem, "c")
        self.ds = []
        self.waited = {e: {} for e in ENGS}

    def dma_stream(self, name, step=16):
        sem = self.stack.enter_context(self.nc.semaphore("d_" + name))
        s = Stream(name, sem, "d", step)
        self.ds.append(s)
        return s

    def _wait(self, eng, stream, val):
        if val <= 0:
            return
        if self.waited[eng].get(stream.name, 0) >= val:
            return
        self.waited[eng][stream.name] = val
        sem = stream.sem
        self.ops[eng].append(lambda e, sem=sem, val=val: e.wait_ge(sem, val))

    def _deps(self, eng, reads, writes):
        mine = self.cs.get(eng)
        for r in reads:
            if r.w is not None:
                s, idx = r.w
                self._wait(eng, s, s.count * s.step if s.kind == "d" else idx + 1)
        for w in writes:
            if w.w is not None:
                s, idx = w.w
                if s is not mine:
                    self._wait(eng, s, s.count * s.step if s.kind == "d" else idx + 1)
            for s, idx in w.rs.values():
                if s is not mine:
                    self._wait(eng, s, s.count * s.step if s.kind == "d" else idx + 1)

    def _mark(self, tag, reads, writes, convert=True):
        s, idx = tag
        for r in reads:
            r.rs[s.name] = tag
            if convert and r.w is not None and r.w[0].kind == "d" and s.kind == "c":
                r.w = tag
        for w in writes:
            w.w = tag
            w.rs = {}

    def op(self, eng, fn, r=(), w=(), inc=True):
        self._deps(eng, r, w)
        st = self.cs[eng]
        idx = st.count
        if inc:
            st.count += 1
            sem = st.sem
            self.ops[eng].append(lambda e, fn=fn, sem=sem: fn(e).then_inc(sem, 1))
        else:
            self.ops[eng].append(lambda e, fn=fn: fn(e))
        self._mark((st, idx), r, w, convert=inc)
        if inc:
            self._yield()

    def dma(self, eng, stream, out, in_, r=(), w=(), **kw):
        self._deps(eng, r, w)
        idx = stream.count
        stream.count += 1
        sem = stream.sem
        self.ops[eng].append(
            lambda e, out=out, in_=in_, sem=sem, kw=kw: e.dma_start(out=out, in_=in_, **kw).then_inc(sem, 16))
        self._mark((stream, idx), r, w)
        self._yield()

    def custom(self, eng, stream, fn, r=(), w=()):
        self._deps(eng, r, w)
        idx = stream.count
        stream.count += 1
        sem = stream.sem
        step = stream.step
        self.ops[eng].append(lambda e, fn=fn, sem=sem, step=step: fn(e).then_inc(sem, step))
        self._mark((stream, idx), r, w)

    def _yield(self):
        il = getattr(self, "_il", None)
        if il is None:
            return
        import threading
        me = il["ids"].get(threading.get_ident())
        if me is None:
            return
        cv = il["cv"]
        with cv:
            if il["alive"][1 - me]:
                il["turn"] = 1 - me
                cv.notify_all()
                while il["turn"] != me and il["alive"][1 - me]:
                    cv.wait()

    def interleave(self, fa, fb):
        import threading
        il = {"cv": threading.Condition(), "turn": 0, "alive": [True, True], "ids": {}, "err": []}
        self._il = il

        def run(idx, f):
            cv = il["cv"]
            with cv:
                il["ids"][threading.get_ident()] = idx
                while il["turn"] != idx and il["alive"][1 - idx]:
                    cv.wait()
            try:
                f()
            except BaseException as ex:
                il["err"].append(ex)
            finally:
                with cv:
                    il["alive"][idx] = False
                    il["turn"] = 1 - idx
                    cv.notify_all()
        ts = [threading.Thread(target=run, args=(0, fa)), threading.Thread(target=run, args=(1, fb))]
        for t in ts:
            t.start()
        for t in ts:
            t.join()
        self._il = None
        if il["err"]:
            raise il["err"][0]

    def barrier(self):
        for e in ENGS:
            for s in list(self.cs.values()) + self.ds:
                if s.kind == "d":
                    self._wait(e, s, s.count * s.step)
                else:
                    self._wait(e, s, s.count)

    def flush(self, block):
        decs = {"pe": block.tensor, "act": block.scalar, "dve": block.vector, "pool": block.gpsimd, "sp": block.sync}
        for e in ENGS:
            ops = self.ops[e]
            self.ops[e] = []
            if not ops:
                continue

            def body(eng, ops=ops):
                for o in ops:
                    o(eng)
            decs[e](body)


def f_act(out, in_, func, **kw):
    return lambda e: e.activation(out=out, in_=in_, func=func, **kw)


def f_ts(out, in0, s1, s2, op0, op1=None, **kw):
    if op1 is None:
        return lambda e: e.tensor_scalar(out=out, in0=in0, scalar1=s1, scalar2=s2, op0=op0, **kw)
    return lambda e: e.tensor_scalar(out=out, in0=in0, scalar1=s1, scalar2=s2, op0=op0, op1=op1, **kw)


def f_tt(out, in0, in1, op):
    return lambda e: e.tensor_tensor(out=out, in0=in0, in1=in1, op=op)


def f_stt(out, in0, scalar, in1, op0, op1, **kw):
    return lambda e: e.scalar_tensor_tensor(out=out, in0=in0, scalar=scalar, in1=in1, op0=op0, op1=op1, **kw)


def f_copy(out, in_):
    return lambda e: e.tensor_copy(out=out, in_=in_)


def f_memset(ap, v):
    return lambda e: e.memset(ap, v)


def f_mm(out, lhsT, rhs, start, stop):
    return lambda e: e.matmul(out, lhsT=lhsT, rhs=rhs, start=start, stop=stop)


def f_tr(out, in_, ident):
    return lambda e: e.transpose(out=out, in_=in_, identity=ident)


def f_red(out, in_, op, axis=AX.X):
    return lambda e: e.tensor_reduce(out=out, in_=in_, axis=axis, op=op)


def f_recip(out, in_):
    return lambda e: e.reciprocal(out=out, in_=in_)


class Ctx:
    def __init__(self, nc, st):
        self.nc, self.st = nc, st
        self.n = 0

    def sb(self, name, shape, dt):
        return self.st.enter_context(self.nc.sbuf_tensor(name, list(shape), dt))

    def ps(self, name):
        return self.st.enter_context(self.nc.psum_tensor(name, [128, 512], F32))


def phase1(nc, P, T, ph):
    with ExitStack() as st:
        C = Ctx(nc, st)
        W = C.sb(ph + "W", [128, 32, NCOL], BF16)
        wst = [C.sb(ph + f"wst{i}", [128, NCOL], F32) for i in range(2)]
        n1w = C.sb(ph + "n1w", [128, 32], F32)
        xt = [C.sb(ph + f"xt{i}", [128, D], F32) for i in range(2)]
        u = [C.sb(ph + f"u{i}", [128, D], BF16) for i in range(2)]
        uT = C.sb(ph + "uT", [128, 32, 512], BF16)
        stat = C.sb(ph + "stat", [128, 8], F32)
        idb = C.sb(ph + "idb", [128, 128], BF16)
        fmst = [C.sb(ph + f"fmst{i}", [128, 512], BF16) for i in range(2)]
        vst = [C.sb(ph + f"vst{i}", [128, 256], BF16) for i in range(2)]
        zst = [C.sb(ph + f"zst{i}", [128, 256], F32) for i in range(2)]
        dst = [C.sb(ph + f"dst{i}", [128, 4], F32) for i in range(2)]
        pT = [C.ps(ph + f"pT{i}") for i in range(2)]
        pF = [C.ps(ph + f"pF{i}") for i in range(2)]
        pA = [C.ps(ph + f"pA{i}") for i in range(2)]
        pB = [C.ps(ph + f"pB{i}") for i in range(2)]
        pTb = [p[:].bitcast(BF16) for p in pT]

        r_W = [Res() for _ in range(32)]
        r_wst = [Res(), Res()]
        r_n1w, r_idb = Res(), Res()
        r_xt = [Res(), Res()]
        r_u = [[Res(), Res()], [Res(), Res()]]
        r_ss = [Res(), Res()]
        r_rs = [Res(), Res()]
        r_uT = [[Res() for _ in range(4)] for _ in range(4)]
        r_pT = [Res(), Res()]
        r_pF = [Res(), Res()]
        r_pA = [Res(), Res()]
        r_pB = [Res(), Res()]
        r_fmst = [Res(), Res()]
        r_vst = [Res(), Res()]
        r_zst = [Res(), Res()]
        r_dst = [Res(), Res()]

        d_w = [P.dma_stream(ph + "w0"), P.dma_stream(ph + "w1")]
        d_x = [P.dma_stream(ph + "x0"), P.dma_stream(ph + "x1")]
        d_c = P.dma_stream(ph + "c")
        d_o = P.dma_stream(ph + "o")

        P.dma("sp", d_c, n1w[:], T["n1w"][:, :], w=[r_n1w])
        P.dma("sp", P.dma_stream(ph + "c2"), idb[:], T["idb"][:, :], w=[r_idb])
        for k in range(32):
            s = k % 2
            P.dma("sp", d_w[s], wst[s][:], T["w_in"][k * 128:(k + 1) * 128, :], w=[r_wst[s]])
            if k % 2 == 0:
                P.op("act", f_act(W[:, k, :], wst[s][:], AF.Copy, scale=n1w[:, k:k + 1]),
                     r=[r_wst[s], r_n1w], w=[r_W[k]])
            else:
                P.op("dve", f_ts(W[:, k, :], wst[s][:], n1w[:, k:k + 1], None, ALU.mult),
                     r=[r_wst[s], r_n1w], w=[r_W[k]])

        ngroups = (NT + 3) // 4
        ntr = 0
        for g in range(ngroups):
            nt = min(4, NT - 4 * g)
            ntok = nt * 128
            tok0 = g * 512
            for t in range(nt):
                f = g * 4 + t
                b, i = divmod(f, TPB)
                s = f % 2
                if i == 0:
                    P.op("pool", f_memset(xt[s][:], 0.0), w=[r_xt[s]])
                    P.dma("sp", d_x[s], xt[s][112:128, :], T["meta"][:, :], w=[r_xt[s]])
                else:
                    row = b * SEQ + (i - 1) * 128
                    P.dma("sp", d_x[s], xt[s][:], T["x"][row:row + 128, :], w=[r_xt[s]])
                P.op("act", f_act(u[s][:], xt[s][:], AF.Square, accum_out=stat[:, s:s + 1]),
                     r=[r_xt[s]], w=[r_u[s][0], r_u[s][1], r_ss[s]])
                P.op("act", f_act(stat[:, 2 + s:3 + s], stat[:, s:s + 1], AF.Sqrt, scale=1.0 / D, bias=EPS),
                     r=[r_ss[s]], w=[r_rs[s]])
                P.op("dve", f_recip(stat[:, 4 + s:5 + s], stat[:, 2 + s:3 + s]), r=[r_rs[s]], w=[r_rs[s]])
                rstd = stat[:, 4 + s:5 + s]
                P.op("act", f_act(u[s][:, 0:2048], xt[s][:, 0:2048], AF.Copy, scale=rstd),
                     r=[r_xt[s], r_rs[s]], w=[r_u[s][0]])
                P.op("dve", f_ts(u[s][:, 2048:4096], xt[s][:, 2048:4096], rstd, None, ALU.mult),
                     r=[r_xt[s], r_rs[s]], w=[r_u[s][1]])
                for kb in range(4):
                    pb = ntr % 2
                    ntr += 1
                    for kk in range(8):
                        k = kb * 8 + kk
                        P.op("pe", f_tr(pTb[pb][:, kk * 128:(kk + 1) * 128], u[s][:, k * 128:(k + 1) * 128], idb[:]),
                             r=[r_u[s][k // 16], r_idb], w=[r_pT[pb]], inc=(kk == 7))
                    dst_ap = uT[:, kb * 8:(kb + 1) * 8, t * 128:(t + 1) * 128]
                    src_ap = pTb[pb][:, :].rearrange("p (a b) -> p a b", a=8)
                    eng = "act" if kb % 2 == 0 else "dve"
                    if eng == "act":
                        P.op("act", f_act(dst_ap, src_ap, AF.Copy), r=[r_pT[pb]], w=[r_uT[t][kb]])
                    else:
                        P.op("dve", f_copy(dst_ap, src_ap), r=[r_pT[pb]], w=[r_uT[t][kb]])
            for j in range(8):
                pb = j % 2
                for k in range(32):
                    P.op("pe", f_mm(pF[pb][:, 0:ntok], W[:, k, j * 128:(j + 1) * 128], uT[:, k, 0:ntok], k == 0, k == 31),
                         r=[r_W[k]] + [r_uT[t][k // 8] for t in range(nt)], w=[r_pF[pb]], inc=(k == 31))
                if j % 2 == 0:
                    P.op("act", f_act(fmst[pb][:, 0:ntok], pF[pb][:, 0:ntok], AF.Copy), r=[r_pF[pb]], w=[r_fmst[pb]])
                else:
                    P.op("dve", f_copy(fmst[pb][:, 0:ntok], pF[pb][:, 0:ntok]), r=[r_pF[pb]], w=[r_fmst[pb]])
                P.dma("sp", d_o, T["fm_scr"][j, :, tok0:tok0 + ntok], fmst[pb][:, 0:ntok], r=[r_fmst[pb]])
            for t in range(nt):
                pb = t % 2
                row = tok0 + t * 128
                for k in range(32):
                    P.op("pe", f_mm(pA[pb][:, 0:260], uT[:, k, t * 128:(t + 1) * 128], W[:, k, 1024:1284], k == 0, k == 31),
                         r=[r_W[k], r_uT[t][k // 8]], w=[r_pA[pb]], inc=False)
                    P.op("pe", f_mm(pB[pb][:, 0:256], uT[:, k, t * 128:(t + 1) * 128], W[:, k, 1284:1540], k == 0, k == 31),
                         r=[r_W[k], r_uT[t][k // 8]], w=[r_pB[pb]], inc=(k == 31))
                P.op("act", f_act(vst[pb][:], pA[pb][:, 0:256], AF.Copy), r=[r_pA[pb]], w=[r_vst[pb]])
                P.op("dve", f_copy(dst[pb][:], pA[pb][:, 256:260]), r=[r_pA[pb]], w=[r_dst[pb]])
                P.op("dve", f_copy(zst[pb][:], pB[pb][:, 0:256]), r=[r_pB[pb]], w=[r_zst[pb]])
                P.dma("sp", d_o, T["v_scr"][row:row + 128, :], vst[pb][:], r=[r_vst[pb]])
                P.dma("sp", d_o, T["dt_scr"][row:row + 128, :], dst[pb][:], r=[r_dst[pb]])
                P.dma("sp", d_o, T["z_scr"][row:row + 128, :], zst[pb][:], r=[r_zst[pb]])
        P.barrier()
        with nc.Block() as block:
            P.flush(block)


PP = {}
_o = 0
for _n, _w in (("convw", 16), ("convb", 4), ("biasT", 256), ("maskT", 256), ("c31", 1), ("alog", 4), ("dtb", 4),
               ("dsk", 4), ("ssmw", 256), ("subw", 256), ("lq1", 128), ("lk1", 128), ("lq2", 128), ("lk2", 128),
               ("tri", 128), ("mneg", 128), ("padm", 1), ("ones", 128)):
    PP[_n] = (_o, _o + _w)
    _o += _w
NPP = _o


def phase2(nc, P, T, ph):
    with ExitStack() as st:
        C = Ctx(nc, st)
        pp = C.sb(ph + "pp", [128, NPP], F32)
        idb = C.sb(ph + "idb", [128, 128], BF16)
        KT = C.sb(ph + "KT", [128, 2, TPB * 128], BF16)
        V = C.sb(ph + "V", [128, TPB, 256], BF16)
        xin = C.sb(ph + "xin", [128, 4, 3 + TPB * 128], BF16)
        HW2 = TPB * 128 // 2
        acc = C.sb(ph + "acc", [128, 2, HW2], F32)
        S = [C.sb(ph + f"S{i}", [128, TPB * 128], F32) for i in range(2)]
        P0 = [C.sb(ph + f"P0{i}", [128, TPB * 128], BF16) for i in range(2)]
        P1 = C.sb(ph + "P1", [128, TPB * 128], BF16)
        aT = C.sb(ph + "aT", [128, TPB, 128], BF16)
        Qt = [C.sb(ph + f"Qt{i}", [128, 2, 128], BF16) for i in range(2)]
        dtr = C.sb(ph + "dtr", [128, TPB, 4], F32)
        dt = C.sb(ph + "dt", [128, TPB, 4], F32)
        adt = C.sb(ph + "adt", [128, TPB, 4], F32)
        sp1 = C.sb(ph + "sp1", [128, TPB, 4], F32)
        sp2 = C.sb(ph + "sp2", [128, TPB, 4], F32)
        sm = C.sb(ph + "sm", [128, 64], F32)
        a_b = C.sb(ph + "a_b", [128, 4], F32)
        biasm = C.sb(ph + "biasm", [128, 256], F32)
        subw8 = C.sb(ph + "subw8", [128, 256], F32)
        lamt = C.sb(ph + "lamt", [128, 128], F32)
        zt = [C.sb(ph + f"zt{i}", [128, 256], F32) for i in range(2)]
        zraw = [C.sb(ph + f"zraw{i}", [128, 256], F32) for i in range(2)]
        ez = C.sb(ph + "ez", [128, 256], F32)
        xs = C.sb(ph + "xs", [128, 256], F32)
        Btok = C.sb(ph + "Btok", [128, 128], BF16)
        Xb = C.sb(ph + "Xb", [128, 4, 64], BF16)
        Xd = C.sb(ph + "Xd", [128, 4, 64], BF16)
        R = C.sb(ph + "R", [128, 4, 128], F32)
        arg = C.sb(ph + "arg", [128, 4, 128], F32)
        dec = C.sb(ph + "dec", [128, 4, 128], F32)
        MT = C.sb(ph + "MT", [128, 4, 128], BF16)
        acs = C.sb(ph + "acs", [128, 16], F32)
        dta = C.sb(ph + "dta", [128, 4], F32)
        y = C.sb(ph + "y", [128, 4, 64], F32)
        ytmp = C.sb(ph + "ytmp", [128, 4, 64], F32)
        g = C.sb(ph + "g", [128, 256], F32)
        gsq = C.sb(ph + "gsq", [128, 256], F32)
        h = C.sb(ph + "h", [128, 4, 64], F32)
        hb = C.sb(ph + "hb", [128, 256], BF16)
        osb = C.sb(ph + "osb", [128, 256], F32)
        osq = C.sb(ph + "osq", [128, 256], F32)
        mixa = C.sb(ph + "mixa", [128, 256], BF16)
        mixs = C.sb(ph + "mixs", [128, 256], BF16)
        mta = [C.sb(ph + f"mta{i}", [128, 2, 128], BF16) for i in range(2)]
        mts = [C.sb(ph + f"mts{i}", [128, 2, 128], BF16) for i in range(2)]
        pS = [C.ps(ph + f"pS{i}") for i in range(2)]
        pT = C.ps(ph + "pT")
        pO = C.ps(ph + "pO")
        pX = C.ps(ph + "pX")
        pC = C.ps(ph + "pC")
        pM = C.ps(ph + "pM")
        pY = C.ps(ph + "pY")
        pTb = pT[:].bitcast(BF16)
        pOb = pO[:].bitcast(BF16)
        pXb = pX[:].bitcast(BF16)

        def col(n, a=None, b=None):
            lo, hi = PP[n]
            if a is None:
                return pp[:, lo:hi]
            return pp[:, lo + a:lo + b]

        r_pp, r_idb = Res(), Res()
        r_KT, r_V, r_xin, r_acc = Res(), Res(), [Res() for _ in range(4)], [Res(), Res()]
        r_S, r_P0, r_P1, r_aT = [Res(), Res()], [Res(), Res()], Res(), Res()
        r_Qt = [Res(), Res()]
        r_dt, r_sm, r_const = Res(), Res(), Res()
        r_zt = [Res(), Res()]
        r_zraw = [Res(), Res()]
        r_pS, r_pT, r_pO, r_pX, r_pC, r_pM, r_pY = [Res(), Res()], Res(), Res(), Res(), Res(), Res(), Res()
        r_xs, r_Btok, r_Xb, r_Xd, r_R, r_arg, r_dec, r_MT, r_acs, r_dta = (Res() for _ in range(10))
        r_y, r_ytmp, r_g, r_gsq, r_h, r_hb, r_osb, r_osq, r_mixa, r_mixs, r_ez = (Res() for _ in range(11))
        r_mta, r_mts = [Res(), Res()], [Res(), Res()]
        r_l = Res()
        r_sm2 = Res()

        d_c = P.dma_stream(ph + "c")
        d_b = P.dma_stream(ph + "b")
        d_b2 = P.dma_stream(ph + "b2")
        d_q = [P.dma_stream(ph + "q0"), P.dma_stream(ph + "q1")]
        d_z = [P.dma_stream(ph + "z0"), P.dma_stream(ph + "z1")]
        d_o = P.dma_stream(ph + "o")

        P.dma("sp", d_c, pp[:], T["pp"][:, :], w=[r_pp])
        P.dma("sp", P.dma_stream(ph + "c2"), idb[:], T["idb"][:, :], w=[r_idb])
        P.op("act", f_act(a_b[:], col("alog"), AF.Exp), r=[r_pp], w=[r_const])
        P.op("dve", f_ts(a_b[:], a_b[:], -1.0, None, ALU.mult), r=[r_const], w=[r_const])
        P.op("dve", f_stt(biasm[:], col("biasT"), col("c31"), col("maskT"), ALU.subtract, ALU.add), r=[r_pp], w=[r_const])
        P.op("dve", f_ts(subw8[:], col("subw"), 1.0 - LAMBDA_INIT, None, ALU.mult), r=[r_pp], w=[r_const])
        P.op("dve", f_tt(lamt[:], col("lq1"), col("lk1"), ALU.mult), r=[r_pp], w=[r_sm])
        P.op("dve", f_red(sm[:, 0:1], lamt[:], ALU.add), r=[r_sm], w=[r_sm])
        P.op("dve", f_tt(lamt[:], col("lq2"), col("lk2"), ALU.mult), r=[r_sm, r_pp], w=[r_sm])
        P.op("dve", f_red(sm[:, 1:2], lamt[:], ALU.add), r=[r_sm], w=[r_sm])
        P.op("act", f_act(sm[:, 2:4], sm[:, 0:2], AF.Exp), r=[r_sm], w=[r_sm])
        P.op("dve", f_tt(sm[:, 4:5], sm[:, 3:4], sm[:, 2:3], ALU.subtract), r=[r_sm], w=[r_sm])
        P.op("dve", f_ts(sm[:, 4:5], sm[:, 4:5], -LAMBDA_INIT, None, ALU.add), r=[r_sm], w=[r_const])
        neglam = sm[:, 4:5]
        nbat = 0 if 'stop_const' in DBG else NB

        for b in range(nbat):
            tb0 = b * TPB * 128
            for m in range(2):
                P.dma("sp", d_b, KT[:, m, :], T["fm_scr"][2 + m, :, tb0:tb0 + TPB * 128], w=[r_KT])
            P.dma("sp", d_b, V[:], T["v_scr"][tb0:tb0 + TPB * 128, :].rearrange("(i p) c -> p i c", p=128), w=[r_V])
            for j in range(4):
                P.op("pool", f_memset(xin[:, j, 0:3], 0.0), w=[r_xin[j]])
                P.dma("sp", d_b2, xin[:, j, 3:], T["fm_scr"][4 + j, :, tb0:tb0 + TPB * 128], w=[r_xin[j]])
            P.dma("sp", d_b2, dtr[:], T["dt_scr"][tb0:tb0 + TPB * 128, :].rearrange("(i p) c -> p i c", p=128), w=[r_dt])
            dtb_b = col("dtb").unsqueeze(1).to_broadcast([128, TPB, 4])
            P.op("dve", f_tt(dtr[:], dtr[:], dtb_b, ALU.add), r=[r_dt, r_pp], w=[r_dt])
            P.op("dve", f_ts(sp1[:], dtr[:], -1.0, None, ALU.mult), r=[r_dt], w=[r_dt])
            P.op("dve", f_tt(sp1[:], sp1[:], dtr[:], ALU.max), r=[r_dt], w=[r_dt])
            P.op("act", f_act(sp2[:], sp1[:], AF.Exp, scale=-1.0), r=[r_dt], w=[r_dt])
            P.op("act", f_act(sp2[:], sp2[:], AF.Ln, bias=1.0), r=[r_dt], w=[r_dt])
            P.op("dve", f_ts(sp1[:], dtr[:], 0.0, None, ALU.max), r=[r_dt], w=[r_dt])
            P.op("dve", f_tt(dt[:], sp1[:], sp2[:], ALU.add), r=[r_dt], w=[r_dt])
            P.op("dve", f_ts(dt[:, 0, :], dt[:, 0, :], col("padm"), None, ALU.mult), r=[r_dt, r_pp], w=[r_dt])
            P.op("dve", f_tt(adt[:], dt[:], a_b[:].unsqueeze(1).to_broadcast([128, TPB, 4]), ALU.mult),
                 r=[r_dt, r_const], w=[r_dt])
            for j in range(4):
                for hf in range(2):
                    c0 = hf * HW2
                    cw = lambda tap, j=j: col("convw", j * 4 + tap, j * 4 + tap + 1)
                    P.op("dve", f_ts(acc[:, hf, :], xin[:, j, c0 + 3:c0 + 3 + HW2], cw(3), col("convb", j, j + 1), ALU.mult, ALU.add),
                         r=[r_xin[j], r_pp], w=[r_acc[hf]])
                    for tap in (2, 1, 0):
                        P.op("dve", f_stt(acc[:, hf, :], xin[:, j, c0 + tap:c0 + tap + HW2], cw(tap), acc[:, hf, :], ALU.mult, ALU.add),
                             r=[r_xin[j], r_acc[hf], r_pp], w=[r_acc[hf]])
                for hf in range(2):
                    c0 = hf * HW2
                    P.op("act", f_act(xin[:, j, c0 + 3:c0 + 3 + HW2], acc[:, hf, :], AF.Silu), r=[r_acc[hf]], w=[r_xin[j]])
            for m in range(2):
                P.op("pool", f_memset(P0[m][:, 0:112], 0.0), w=[r_P0[m]])

            nq = 0
            for i in range(0 if 'stop_conv' in DBG else TPB):
                def ssd_block(i=i, b=b, tb0=tb0):
                    tk = slice(i * 128, (i + 1) * 128)
                    zs = i % 2
                    if i >= 1:
                        zrow = tb0 + i * 128
                        P.dma("sp", d_z[zs], zraw[zs][:], T["z_scr"][zrow:zrow + 128, :], w=[r_zraw[zs]])
                        P.op("act", f_act(zt[zs][:], zraw[zs][:], AF.Copy), r=[r_zraw[zs]], w=[r_zt[zs]])
                    for jj in range(3):
                        P.op("pe", f_tr(pXb[:, jj * 128:(jj + 1) * 128], xin[:, jj, 3 + i * 128:3 + (i + 1) * 128], idb[:]),
                             r=[r_xin[jj], r_idb], w=[r_pX], inc=(jj == 2))
                    P.op("act", f_act(xs[:], pXb[:, 0:256], AF.Copy), r=[r_pX], w=[r_xs])
                    P.op("act", f_act(Btok[:], pXb[:, 256:384], AF.Copy), r=[r_pX], w=[r_Btok])
                    dt_b = dt[:, i, :].unsqueeze(2).to_broadcast([128, 4, 64])
                    P.op("dve", f_tt(Xb[:], pXb[:, 0:256].rearrange("p (r c) -> p r c", r=4), dt_b, ALU.mult),
                         r=[r_pX, r_dt], w=[r_Xb])
                    if SSD_LIM >= 2:
                        P.op("pool", f_tt(R[:], col("tri").unsqueeze(1).to_broadcast([128, 4, 128]),
                                          adt[:, i, :].unsqueeze(2).to_broadcast([128, 4, 128]), ALU.mult),
                             r=[r_pp, r_dt], w=[r_R])
                        P.op("pe", f_mm(pC[:, 0:512], col("ones"), R[:].rearrange("p r l -> p (r l)"), True, True),
                             r=[r_pp, r_R], w=[r_pC])
                        P.op("pe", f_mm(pM[:, 0:4], col("tri"), adt[:, i, :], True, True), r=[r_pp, r_dt], w=[r_pM])
                        P.op("dve", f_copy(acs[:, 0:4], pM[:, 0:4]), r=[r_pM], w=[r_acs])
                        pC3 = pC[:, 0:512].rearrange("p (r l) -> p r l", r=4)
                    if SSD_LIM >= 3:
                        P.op("dve", f_tt(arg[:], pC3, acs[:, 0:4].unsqueeze(2).to_broadcast([128, 4, 128]), ALU.subtract),
                             r=[r_pC, r_acs], w=[r_arg])
                        P.op("dve", f_tt(arg[:], arg[:], col("mneg").unsqueeze(1).to_broadcast([128, 4, 128]), ALU.add),
                             r=[r_arg, r_pp], w=[r_arg])
                        P.op("act", f_act(dec[:], arg[:], AF.Exp), r=[r_arg], w=[r_dec])
                        P.op("act", f_act(acs[:, 4:8], acs[:, 0:4], AF.Exp), r=[r_acs], w=[r_acs])
                        P.op("dve", f_tt(dta[:], pC3[:, :, 127], acs[:, 0:4], ALU.subtract), r=[r_pC, r_acs], w=[r_dta])
                        P.op("act", f_act(acs[:, 8:12], dta[:], AF.Exp), r=[r_dta], w=[r_acs])
                        P.op("act", f_act(acs[:, 12:16], pC3[:, :, 127], AF.Exp), r=[r_pC], w=[r_acs])
                    if SSD_LIM >= 4:
                        P.op("pe", f_mm(pM[:, 128:256], xin[:, 2, 3 + i * 128:3 + (i + 1) * 128],
                                        xin[:, 3, 3 + i * 128:3 + (i + 1) * 128], True, True),
                             r=[r_xin[2], r_xin[3]], w=[r_pM])
                    if SSD_LIM >= 5:
                        if i >= 1:
                            P.op("dve", f_tt(MT[:], dec[:], pM[:, 128:256].unsqueeze(1).to_broadcast([128, 4, 128]), ALU.mult),
                                 r=[r_dec, r_pM], w=[r_MT])
                            for rr in range(4):
                                P.op("pe", f_mm(pY[:, rr * 64:(rr + 1) * 64], MT[:, rr, :], Xb[:, rr, :], True, True),
                                     r=[r_MT, r_Xb], w=[r_pY], inc=False)
                            P.op("pe", f_mm(pY[:, 256:512], xin[:, 3, 3 + i * 128:3 + (i + 1) * 128], hb[:], True, True),
                                 r=[r_xin[3], r_hb], w=[r_pY])
                            eb = acs[:, 4:8].unsqueeze(2).to_broadcast([128, 4, 64])
                            P.op("dve", f_tt(y[:], pY[:, 256:512].rearrange("p (r c) -> p r c", r=4), eb, ALU.mult),
                                 r=[r_pY, r_acs], w=[r_y])
                            P.op("dve", f_tt(y[:], y[:], pY[:, 0:256].rearrange("p (r c) -> p r c", r=4), ALU.add),
                                 r=[r_y, r_pY], w=[r_y])
                            P.op("pool", f_tt(ytmp[:], xs[:].rearrange("p (r c) -> p r c", r=4),
                                              col("dsk").unsqueeze(2).to_broadcast([128, 4, 64]), ALU.mult),
                                 r=[r_xs, r_pp], w=[r_ytmp])
                            P.op("dve", f_tt(y[:], y[:], ytmp[:], ALU.add), r=[r_y, r_ytmp], w=[r_y])
                    if SSD_LIM >= 6:
                        P.op("dve", f_tt(Xd[:], Xb[:], acs[:, 8:12].unsqueeze(2).to_broadcast([128, 4, 64]), ALU.mult),
                             r=[r_Xb, r_acs], w=[r_Xd])
                        P.op("pe", f_mm(pM[:, 256:512], Btok[:], Xd[:].rearrange("p r c -> p (r c)"), True, True),
                             r=[r_Btok, r_Xd], w=[r_pM])
                        pM3 = pM[:, 256:512].rearrange("p (r c) -> p r c", r=4)
                        if i == 0:
                            P.op("dve", f_copy(h[:], pM3), r=[r_pM], w=[r_h])
                        else:
                            P.op("dve", f_tt(h[:], h[:], acs[:, 12:16].unsqueeze(2).to_broadcast([128, 4, 64]), ALU.mult),
                                 r=[r_h, r_acs], w=[r_h])
                            P.op("dve", f_tt(h[:], h[:], pM3, ALU.add), r=[r_h, r_pM], w=[r_h])
                        P.op("act", f_act(hb[:], h[:].rearrange("p r c -> p (r c)"), AF.Copy), r=[r_h], w=[r_hb])
                    if SSD_LIM >= 7:
                        if i >= 1:
                            if S7 >= 1:
                                P.op("act", f_act(ez[:], zt[zs][:], AF.Exp, scale=-1.0), r=[r_zt[zs]], w=[r_ez])
                            if S7 >= 2:
                                P.op("dve", f_ts(ez[:], ez[:], 1.0, None, ALU.add), r=[r_ez], w=[r_ez])
                            if S7 >= 3:
                                P.op("dve", f_recip(ez[:], ez[:]), r=[r_ez], w=[r_ez])
                            if S7 >= 4:
                                P.op("dve", f_tt(g[:], y[:].rearrange("p r c -> p (r c)"), zt[zs][:], ALU.mult), r=[r_y, r_zt[zs]], w=[r_g])
                            if S7 >= 5:
                                P.op("dve", f_tt(g[:], g[:], ez[:], ALU.mult), r=[r_g, r_ez], w=[r_g])
                            if S7 >= 6:
                                P.op("pool", f_tt(gsq[:], g[:], g[:], ALU.mult), r=[r_g], w=[r_gsq])
                            if S7 >= 7:
                                P.op("dve", f_red(sm[:, 16:17], gsq[:], ALU.add), r=[r_gsq], w=[r_sm])
                            if S7 >= 8:
                                P.op("dve", f_ts(sm[:, 16:17], sm[:, 16:17], 1.0 / 256, EPS, ALU.mult, ALU.add), r=[r_sm], w=[r_sm])
                                if "s7g" in DBG:
                                    P.op("act", f_act(dta[:, 2:3], g[:, 0:1], AF.Copy), r=[r_g], w=[Res()])
                                elif "s7ez" in DBG:
                                    P.op("act", f_act(dta[:, 2:3], ez[:, 0:1], AF.Copy), r=[r_ez], w=[Res()])
                                elif "s7dummy" in DBG:
                                    P.op("act", f_act(dta[:, 2:3], col("c31"), AF.Copy), r=[r_pp], w=[Res()])
                                    P.op("act", f_act(sm[:, 17:18], sm[:, 16:17], AF.Sqrt), r=[r_sm], w=[r_sm])
                                elif "s7nodep" in DBG:
                                    P.op("act", f_act(dta[:, 1:2], col("c31"), AF.Copy), r=[r_pp], w=[r_dta])
                                elif "s7sep" in DBG:
                                    P.op("dve", f_copy(dta[:, 0:1], sm[:, 16:17]), r=[r_sm], w=[r_dta])
                                    P.op("act", f_act(dta[:, 1:2], dta[:, 0:1], AF.Sqrt), r=[r_dta], w=[r_dta])
                                elif "s7dve" in DBG:
                                    P.op("dve", f_copy(sm[:, 17:18], sm[:, 16:17]), r=[r_sm], w=[r_sm])
                                elif "s7other" in DBG:
                                    P.op("act", f_act(acs[:, 4:5], sm[:, 16:17], AF.Copy), r=[r_sm], w=[r_acs])
                                else:
                                    P.op("act", f_act(sm[:, 17:18], sm[:, 16:17], AF.Copy if "s7copy" in DBG else AF.Sqrt), r=[r_sm], w=[r_sm])
                            if S7 >= 9:
                                P.op("dve", f_recip(sm[:, 18:19], sm[:, 17:18]), r=[r_sm], w=[r_sm])
                            if S7 >= 10:
                                P.op("dve", f_stt(mixs[:], g[:], sm[:, 18:19], col("ssmw"), ALU.mult, ALU.mult), r=[r_g, r_sm, r_pp], w=[r_mixs])
                            ms = i % 2
                            for jj in range(0 if 'nomixtr' in DBG else 2):
                                P.op("pe", f_tr(pXb[:, 512 + jj * 128:512 + (jj + 1) * 128], mixs[:, jj * 128:(jj + 1) * 128], idb[:]),
                                     r=[r_mixs, r_idb], w=[r_pX], inc=(jj == 1))
                            P.op("act", f_act(mts[ms][:], pXb[:, 512:768].rearrange("p (a c) -> p a c", a=2), AF.Copy),
                                 r=[r_pX], w=[r_mts[ms]])
                            otok = b * SEQ + (i - 1) * 128
                            if 'nomixdma' not in DBG:
                                oj, oo = divmod(otok, 1024)
                                P.dma("sp", d_o, T["mixT"][oj * 512 + 256:oj * 512 + 512, oo:oo + 128].rearrange("(a p) t -> p a t", p=128),
                                      mts[ms][:], r=[r_mts[ms]], w=[T["_r_mix"]])


                def attn_block(i=i, b=b, tb0=tb0):
                    nonlocal nq
                    if i == 0 or 'noattn' in DBG:
                        return
                    qs = i % 2
                    qcol = tb0 + i * 128
                    for m in range(2):
                        P.dma("sp", d_q[qs], Qt[qs][:, m, :], T["fm_scr"][m, :, qcol:qcol + 128], w=[r_Qt[qs]])
                    kend = (i + 1) * 128
                    pb = i % 2
                    nev = 0
                    for m in range(2):
                        for k0 in range(0, kend, 512):
                            wd = min(512, kend - k0)
                            sb_ = nq % 2
                            nq += 1
                            P.op("pe", f_mm(pS[sb_][:, 0:wd], Qt[qs][:, m, :], KT[:, m, k0:k0 + wd], True, True),
                                 r=[r_Qt[qs], r_KT], w=[r_pS[sb_]])
                            if nev % 2 == 0:
                                P.op("act", f_act(S[m][:, k0:k0 + wd], pS[sb_][:, 0:wd], AF.Copy, scale=SCALE),
                                     r=[r_pS[sb_]], w=[r_S[m]])
                            else:
                                P.op("dve", f_ts(S[m][:, k0:k0 + wd], pS[sb_][:, 0:wd], SCALE, None, ALU.mult),
                                     r=[r_pS[sb_]], w=[r_S[m]])
                            nev += 1
                        P.op("dve", f_tt(S[m][:, kend - 256:kend], S[m][:, kend - 256:kend], biasm[:], ALU.add),
                             r=[r_S[m], r_const], w=[r_S[m]])
                        P.op("dve", f_red(sm[:, 20 + m:21 + m], S[m][:, 112:kend], ALU.max), r=[r_S[m]], w=[r_l])
                        P.op("dve", f_ts(sm[:, 22 + m:23 + m], sm[:, 20 + m:21 + m], -1.0, None, ALU.mult), r=[r_l], w=[r_l])
                        dstP = P0[pb] if m == 0 else P1
                        P.op("act", f_act(dstP[:, 112:kend], S[m][:, 112:kend], AF.Exp, bias=sm[:, 22 + m:23 + m],
                                          accum_out=sm[:, 24 + m:25 + m]),
                             r=[r_S[m], r_l], w=[(r_P0[pb] if m == 0 else r_P1), r_l])
                    P.op("dve", f_recip(sm[:, 26:28], sm[:, 24:26]), r=[r_l], w=[r_l])
                    P.op("dve", f_tt(sm[:, 28:29], sm[:, 24:25], sm[:, 27:28], ALU.mult), r=[r_l], w=[r_l])
                    P.op("dve", f_tt(sm[:, 29:30], sm[:, 28:29], neglam, ALU.mult), r=[r_l, r_const], w=[r_l])
                    P.op("dve", f_stt(P0[pb][:, 112:kend], P1[:, 112:kend], sm[:, 29:30], P0[pb][:, 112:kend], ALU.mult, ALU.add),
                         r=[r_P1, r_P0[pb], r_l], w=[r_P0[pb]])
                    nkt = i + 1
                    for kg in range(0, nkt, 8):
                        ng = min(8, nkt - kg)
                        for kk in range(ng):
                            kt = kg + kk
                            P.op("pe", f_tr(pTb[:, kk * 128:(kk + 1) * 128], P0[pb][:, kt * 128:(kt + 1) * 128], idb[:]),
                                 r=[r_P0[pb], r_idb], w=[r_pT], inc=(kk == ng - 1))
                        src_ap = pTb[:, 0:ng * 128].rearrange("p (a c) -> p a c", a=ng)
                        if (kg // 8) % 2 == 0:
                            P.op("act", f_act(aT[:, kg:kg + ng, :], src_ap, AF.Copy), r=[r_pT], w=[r_aT])
                        else:
                            P.op("dve", f_copy(aT[:, kg:kg + ng, :], src_ap), r=[r_pT], w=[r_aT])
                    for kt in range(nkt):
                        P.op("pe", f_mm(pO[:, 0:256], aT[:, kt, :], V[:, kt, :], kt == 0, kt == nkt - 1),
                             r=[r_aT, r_V], w=[r_pO], inc=(kt == nkt - 1))
                    P.op("dve", f_ts(osb[:], pO[:, 0:256], sm[:, 26:27], None, ALU.mult), r=[r_pO, r_l], w=[r_osb])
                    P.op("pool", f_tt(osq[:], osb[:], osb[:], ALU.mult), r=[r_osb], w=[r_osq])
                    P.op("dve", f_red(sm[:, 32:33], osq[:], ALU.add), r=[r_osq], w=[r_sm2])
                    P.op("dve", f_ts(sm[:, 32:33], sm[:, 32:33], 1.0 / 256, EPS, ALU.mult, ALU.add), r=[r_sm2], w=[r_sm2])
                    P.op("act", f_act(sm[:, 33:34], sm[:, 32:33], AF.Sqrt), r=[r_sm2], w=[r_sm2])
                    P.op("dve", f_recip(sm[:, 34:35], sm[:, 33:34]), r=[r_sm2], w=[r_sm2])
                    P.op("dve", f_stt(mixa[:], osb[:], sm[:, 34:35], subw8[:], ALU.mult, ALU.mult), r=[r_osb, r_sm2, r_const], w=[r_mixa])
                    ma = i % 2
                    for jj in range(2):
                        P.op("pe", f_tr(pOb[:, 512 + jj * 128:512 + (jj + 1) * 128], mixa[:, jj * 128:(jj + 1) * 128], idb[:]),
                             r=[r_mixa, r_idb], w=[r_pO], inc=(jj == 1))
                    P.op("act", f_act(mta[ma][:], pOb[:, 512:768].rearrange("p (a c) -> p a c", a=2), AF.Copy),
                         r=[r_pO], w=[r_mta[ma]])
                    otok = b * SEQ + (i - 1) * 128
                    oj, oo = divmod(otok, 1024)
                    P.dma("sp", d_o, T["mixT"][oj * 512:oj * 512 + 256, oo:oo + 128].rearrange("(a p) t -> p a t", p=128), mta[ma][:],
                          r=[r_mta[ma]], w=[T["_r_mix"]])

                P.interleave(ssd_block, attn_block)
        P.barrier()
        with nc.Block() as block:
            P.flush(block)


def phase3a(nc, P, T, ph):
    with ExitStack() as st:
        C = Ctx(nc, st)
        mT = C.sb(ph + "mT", [128, 32, 1024], BF16)
        Wb = [C.sb(ph + f"Wb{i}", [128, 32, 512], BF16) for i in range(2)]
        wst = [C.sb(ph + f"wst{i}", [128, 4, 512], F32) for i in range(2)]
        xr = [C.sb(ph + f"xr{i}", [128, 512], F32) for i in range(2)]
        hb_ = [C.sb(ph + f"hb{i}", [128, 512], F32) for i in range(2)]
        pO = [C.ps(ph + f"pO{i}") for i in range(4)]
        r_mT = [Res() for _ in range(32)]
        r_Wb = [[Res() for _ in range(8)] for _ in range(2)]
        r_wst, r_xr, r_hb = [Res(), Res()], [Res(), Res()], [Res(), Res()]
        r_pO = [Res() for _ in range(4)]
        d_m = P.dma_stream(ph + "m")
        d_w = [P.dma_stream(ph + "w0"), P.dma_stream(ph + "w1")]
        d_x = [P.dma_stream(ph + "x0"), P.dma_stream(ph + "x1")]
        d_o = P.dma_stream(ph + "o")
        for kc in range(32):
            P.dma("sp", d_m, mT[:, kc, :], T["mT_in"][kc * 128:(kc + 1) * 128, :], w=[r_mT[kc]])
        wv = T["w_out"].rearrange("(kc p) n -> p kc n", p=128)
        n = 0
        nw = 0
        for dg in range(8):
            ws = dg % 2
            for kq in range(8):
                ss = nw % 2
                nw += 1
                P.dma("sp", d_w[ss], wst[ss][:], wv[:, kq * 4:(kq + 1) * 4, dg * 512:(dg + 1) * 512], w=[r_wst[ss]])
                if ss == 0:
                    P.op("act", f_act(Wb[ws][:, kq * 4:(kq + 1) * 4, :], wst[ss][:], AF.Copy), r=[r_wst[ss]], w=[r_Wb[ws][kq]])
                else:
                    P.op("dve", f_copy(Wb[ws][:, kq * 4:(kq + 1) * 4, :], wst[ss][:]), r=[r_wst[ss]], w=[r_Wb[ws][kq]])
            for tt in range(8):
                pb = n % 4
                xs_ = n % 2
                n += 1
                P.dma("sp", d_x[xs_], xr[xs_][:], T["xres"][tt * 128:(tt + 1) * 128, dg * 512:(dg + 1) * 512], w=[r_xr[xs_]])
                for kc in range(32):
                    P.op("pe", f_mm(pO[pb][:, :], mT[:, kc, tt * 128:(tt + 1) * 128], Wb[ws][:, kc, :], kc == 0, kc == 31),
                         r=[r_mT[kc], r_Wb[ws][kc // 4]], w=[r_pO[pb]], inc=(kc == 31))
                P.op("dve", f_tt(hb_[xs_][:], pO[pb][:, :], xr[xs_][:], ALU.add), r=[r_pO[pb], r_xr[xs_]], w=[r_hb[xs_]])
                P.dma("sp", d_o, T["h2"][tt * 128:(tt + 1) * 128, dg * 512:(dg + 1) * 512], hb_[xs_][:], r=[r_hb[xs_]],
                      w=[T["_r_h2"]])
        P.barrier()
        with nc.Block() as block:
            P.flush(block)


def phase3b(nc, P, T, ph):
    with ExitStack() as st:
        C = Ctx(nc, st)
        ht = [C.sb(ph + f"ht{i}", [128, D], F32) for i in range(2)]
        u2f = C.sb(ph + "u2f", [128, D], F32)
        u2h = [C.sb(ph + f"u2h{i}", [128, D], BF16) for i in range(2)]
        u2l = C.sb(ph + "u2l", [128, D], BF16)
        n2w = C.sb(ph + "n2w", [128, D], F32)
        uT = C.sb(ph + "uT", [128, 2, 32, 128], BF16)
        Wrf = C.sb(ph + "Wrf", [128, 32, 72], F32)
        Wrh = C.sb(ph + "Wrh", [128, 32, 72], BF16)
        Wrl = C.sb(ph + "Wrl", [128, 32, 72], BF16)
        Wrt = C.sb(ph + "Wrt", [128, 32, 72], F32)
        rb = C.sb(ph + "rb", [128, 72], F32)
        io8 = C.sb(ph + "io8", [128, 8], F32)
        idb = C.sb(ph + "idb", [128, 128], BF16)
        lg = C.sb(ph + "lg", [128, 72], F32)
        rt = [C.sb(ph + f"rt{i}", [128, 8], F32) for i in range(2)]
        stat = C.sb(ph + "stat", [128, 8], F32)
        q = C.sb(ph + "q", [128, 64], F32)
        goh = C.sb(ph + "goh", [128, 8], F32)
        ge = C.sb(ph + "ge", [128, 8], F32)
        el3 = C.sb(ph + "el3", [128, 8, 8], F32)
        els = C.sb(ph + "els", [128, 8], F32)
        msk = C.sb(ph + "msk", [128, 8], F32)
        oh1 = C.sb(ph + "oh1", [128, 8], F32)
        oh2 = C.sb(ph + "oh2", [128, 8], F32)
        tmp8 = C.sb(ph + "tmp8", [128, 8], F32)
        pT = [C.ps(ph + f"pT{i}") for i in range(2)]
        pL = C.ps(ph + "pL")
        pTb = [p[:].bitcast(BF16) for p in pT]
        r_ht, r_u2h, r_rt = [Res(), Res()], [Res(), Res()], [Res(), Res()]
        r_u2f, r_u2l, r_st, r_pL, r_lg, r_q = Res(), Res(), Res(), Res(), Res(), Res()
        r_uT = [[Res() for _ in range(4)] for _ in range(2)]
        r_pT = [Res(), Res()]
        r_n2w, r_wr, r_rb, r_idb, r_io = Res(), Res(), Res(), Res(), Res()
        d_h = [P.dma_stream(ph + "h0"), P.dma_stream(ph + "h1")]
        d_o = P.dma_stream(ph + "o")
        P.dma("sp", P.dma_stream(ph + "c1"), n2w[:], T["n2w"][:, :], w=[r_n2w])
        P.dma("sp", P.dma_stream(ph + "c2"), Wrf[:], T["wr"].rearrange("(kc p) n -> p kc n", p=128), w=[r_wr])
        P.dma("sp", P.dma_stream(ph + "c3"), rb[:], T["rb"][:, :], w=[r_rb])
        P.dma("sp", P.dma_stream(ph + "c4"), idb[:], T["idb"][:, :], w=[r_idb])
        P.dma("sp", P.dma_stream(ph + "c5"), io8[:], T["io8"][:, :], w=[r_io])
        P.op("dve", f_copy(Wrh[:], Wrf[:]), r=[r_wr], w=[r_wr])
        P.op("dve", f_copy(Wrt[:], Wrh[:]), r=[r_wr], w=[r_wr])
        P.op("dve", f_tt(Wrt[:], Wrf[:], Wrt[:], ALU.subtract), r=[r_wr], w=[r_wr])
        P.op("dve", f_copy(Wrl[:], Wrt[:]), r=[r_wr], w=[r_wr])
        ntr = 0
        for tt in range(8):
            s = tt % 2
            P.dma("sp", d_h[s], ht[s][:], T["h2"][tt * 128:(tt + 1) * 128, :], r=[T["_r_h2"]], w=[r_ht[s]])
            P.op("act", f_act(u2f[:], ht[s][:], AF.Square, accum_out=stat[:, 0:1]), r=[r_ht[s]], w=[r_u2f, r_st])
            P.op("act", f_act(stat[:, 1:2], stat[:, 0:1], AF.Sqrt, scale=1.0 / D, bias=EPS), r=[r_st], w=[r_st])
            P.op("dve", f_recip(stat[:, 2:3], stat[:, 1:2]), r=[r_st], w=[r_st])
            P.op("dve", f_stt(u2f[:], ht[s][:], stat[:, 2:3], n2w[:], ALU.mult, ALU.mult), r=[r_ht[s], r_st, r_n2w], w=[r_u2f])
            P.op("act", f_act(u2h[s][:], u2f[:], AF.Copy), r=[r_u2f], w=[r_u2h[s]])
            P.dma("sp", d_o, T["u2"][tt * 128:(tt + 1) * 128, :], u2h[s][:], r=[r_u2h[s]])
            P.op("dve", f_tt(u2f[:], u2f[:], u2h[s][:], ALU.subtract), r=[r_u2f, r_u2h[s]], w=[r_u2f])
            P.op("act", f_act(u2l[:], u2f[:], AF.Copy), r=[r_u2f], w=[r_u2l])
            for part, src, r_src in ((0, u2h[s], r_u2h[s]), (1, u2l, r_u2l)):
                for kb in range(4):
                    pb = ntr % 2
                    ntr += 1
                    for kk in range(8):
                        k = kb * 8 + kk
                        P.op("pe", f_tr(pTb[pb][:, kk * 128:(kk + 1) * 128], src[:, k * 128:(k + 1) * 128], idb[:]),
                             r=[r_src, r_idb], w=[r_pT[pb]], inc=(kk == 7))
                    dst_ap = uT[:, part, kb * 8:(kb + 1) * 8, :]
                    src_ap = pTb[pb][:, :].rearrange("p (a b) -> p a b", a=8)
                    if kb % 2 == 0:
                        P.op("act", f_act(dst_ap, src_ap, AF.Copy), r=[r_pT[pb]], w=[r_uT[part][kb]])
                    else:
                        P.op("dve", f_copy(dst_ap, src_ap), r=[r_pT[pb]], w=[r_uT[part][kb]])
            combos = [(0, Wrh), (1, Wrh), (0, Wrl)]
            nmm = 0
            for part, Wm in combos:
                for k in range(32):
                    P.op("pe", f_mm(pL[:, 0:72], uT[:, part, k, :], Wm[:, k, :], nmm == 0, nmm == 95),
                         r=[r_uT[part][k // 8], r_wr], w=[r_pL], inc=(nmm == 95))
                    nmm += 1
            P.op("dve", f_tt(lg[:], pL[:, 0:72], rb[:], ALU.add), r=[r_pL, r_rb], w=[r_lg])
            gl = lg[:, 0:8]
            P.op("dve", f_red(q[:, 0:1], gl, ALU.max), r=[r_lg], w=[r_q])
            P.op("dve", f_ts(goh[:], gl, q[:, 0:1], None, ALU.is_equal), r=[r_lg, r_q], w=[r_q])
            P.op("dve", f_ts(q[:, 1:2], q[:, 0:1], -1.0, None, ALU.mult), r=[r_q], w=[r_q])
            P.op("act", f_act(ge[:], gl, AF.Exp, bias=q[:, 1:2], accum_out=q[:, 2:3]), r=[r_lg, r_q], w=[r_q])
            P.op("dve", f_recip(q[:, 3:4], q[:, 2:3]), r=[r_q], w=[r_q])
            P.op("dve", f_tt(el3[:], lg[:, 8:72].rearrange("p (g e) -> p g e", g=8),
                             goh[:].unsqueeze(2).to_broadcast([128, 8, 8]), ALU.mult), r=[r_lg, r_q], w=[r_q])
            P.op("dve", f_red(els[:], el3[:].rearrange("p g e -> p e g"), ALU.add), r=[r_q], w=[r_q])
            P.op("dve", f_red(q[:, 4:5], els[:], ALU.max), r=[r_q], w=[r_q])
            P.op("dve", f_ts(oh1[:], els[:], q[:, 4:5], None, ALU.is_equal), r=[r_q], w=[r_q])
            P.op("dve", f_stt(msk[:], oh1[:], NEG, els[:], ALU.mult, ALU.add), r=[r_q], w=[r_q])
            P.op("dve", f_red(q[:, 5:6], msk[:], ALU.max), r=[r_q], w=[r_q])
            P.op("dve", f_ts(oh2[:], msk[:], q[:, 5:6], None, ALU.is_equal), r=[r_q], w=[r_q])
            P.op("dve", f_tt(q[:, 6:7], q[:, 5:6], q[:, 4:5], ALU.subtract), r=[r_q], w=[r_q])
            P.op("act", f_act(q[:, 7:8], q[:, 6:7], AF.Exp), r=[r_q], w=[r_q])
            P.op("dve", f_ts(q[:, 8:9], q[:, 7:8], 1.0, None, ALU.add), r=[r_q], w=[r_q])
            P.op("dve", f_recip(q[:, 9:10], q[:, 8:9]), r=[r_q], w=[r_q])
            P.op("dve", f_tt(rt[s][:, 2:3], q[:, 3:4], q[:, 9:10], ALU.mult), r=[r_q], w=[r_rt[s]])
            P.op("dve", f_tt(rt[s][:, 3:4], rt[s][:, 2:3], q[:, 7:8], ALU.mult), r=[r_q, r_rt[s]], w=[r_rt[s]])
            P.op("dve", f_tt(tmp8[:], goh[:], io8[:], ALU.mult), r=[r_q, r_io], w=[r_q])
            P.op("dve", f_red(q[:, 10:11], tmp8[:], ALU.add), r=[r_q], w=[r_q])
            P.op("dve", f_tt(tmp8[:], oh1[:], io8[:], ALU.mult), r=[r_q, r_io], w=[r_q])
            P.op("dve", f_red(q[:, 11:12], tmp8[:], ALU.add), r=[r_q], w=[r_q])
            P.op("dve", f_tt(tmp8[:], oh2[:], io8[:], ALU.mult), r=[r_q, r_io], w=[r_q])
            P.op("dve", f_red(q[:, 12:13], tmp8[:], ALU.add), r=[r_q], w=[r_q])
            P.op("dve", f_stt(rt[s][:, 0:1], q[:, 10:11], 8.0, q[:, 11:12], ALU.mult, ALU.add), r=[r_q, r_rt[s]], w=[r_rt[s]])
            P.op("dve", f_stt(rt[s][:, 1:2], q[:, 10:11], 8.0, q[:, 12:13], ALU.mult, ALU.add), r=[r_q, r_rt[s]], w=[r_rt[s]])
            P.op("dve", f_copy(rt[s][:, 4:8], lg[:, 0:4]), r=[r_lg, r_rt[s]], w=[r_rt[s]])
            P.dma("sp", d_o, T["route"][tt * 128:(tt + 1) * 128, :], rt[s][:], r=[r_rt[s]])
        P.barrier()
        with nc.Block() as block:
            P.flush(block)


def build_L2():
    nc = bass.Bass("TRN2", target_bir_lowering=False)
    T = {}
    for name, shape, dt in (("mT_in", [D, 1024], BF16), ("w_out", [D, D], F32), ("xres", [1024, D], F32),
                            ("n2w", [128, D], F32), ("wr", [D, 72], F32), ("rb", [128, 72], F32),
                            ("idb", [128, 128], BF16), ("io8", [128, 8], F32)):
        T[name] = nc.dram_tensor(name, list(shape), dt, kind="ExternalInput").ap()
    for name, shape, dt in (("h2", [1024, D], F32), ("u2", [1024, D], BF16), ("route", [1024, 8], F32)):
        T[name] = nc.dram_tensor(name, list(shape), dt, kind="ExternalOutput").ap()
    T["_r_h2"] = Res()
    with ExitStack() as stack:
        P = Prog(nc, stack)
        phase3a(nc, P, T, "a")
        phase3b(nc, P, T, "b")
    return nc


def build_L3(CAP):
    CH = min(CAP, 384)
    assert CAP % CH == 0 and CH % 128 == 0
    nch = CAP // CH
    nst = CH // 128
    nc = bass.Bass("TRN2", target_bir_lowering=False)
    T = {}
    for name, shape, dt in (("wg", [8, D, 768], F32), ("wu", [8, D, 768], F32), ("wd", [8, 768, D], F32),
                            ("xgT", [8, D, CAP], BF16), ("wts", [128, 8 * (CAP // 128)], F32)):
        T[name] = nc.dram_tensor(name, list(shape), dt, kind="ExternalInput").ap()
    T["y"] = nc.dram_tensor("y", [8, CAP, D], BF16, kind="ExternalOutput").ap()
    with ExitStack() as stack:
        P = Prog(nc, stack)
        C = Ctx(nc, stack)
        xT = C.sb("xT", [128, 32, CH], BF16)
        Wg = C.sb("Wg", [128, 32, 768], BF16)
        Wu = C.sb("Wu", [128, 32, 768], BF16)
        Wd = C.sb("Wd", [128, 6, D], BF16)
        gst = [C.sb(f"gst{i}", [128, 768], F32) for i in range(2)]
        ust = [C.sb(f"ust{i}", [128, 768], F32) for i in range(2)]
        dstg = [C.sb(f"dstg{i}", [128, 1024], F32) for i in range(2)]
        hT = C.sb("hT", [128, 6, CH], BF16)
        tmp = [C.sb(f"tmp{i}", [128, CH], F32) for i in range(2)]
        ys = [C.sb(f"ys{i}", [128, 512], BF16) for i in range(2)]
        wts = C.sb("wts_sb", [128, 8 * (CAP // 128)], F32)
        pG = [C.ps(f"pG{i}") for i in range(3)]
        pU = [C.ps(f"pU{i}") for i in range(3)]
        pD = [C.ps(f"pD{i}") for i in range(2)]
        r_xT, r_hT, r_wts = Res(), [Res() for _ in range(6)], Res()
        r_Wg, r_Wu = [Res() for _ in range(32)], [Res() for _ in range(32)]
        r_Wd = [[Res() for _ in range(4)] for _ in range(6)]
        r_gst, r_ust, r_dstg = [Res(), Res()], [Res(), Res()], [Res(), Res()]
        r_tmp, r_ys = [Res(), Res()], [Res(), Res()]
        r_pG, r_pU, r_pD = [Res() for _ in range(3)], [Res() for _ in range(3)], [Res(), Res()]
        d_x = P.dma_stream("x")
        d_g = [P.dma_stream("g0"), P.dma_stream("g1")]
        d_u = [P.dma_stream("u0"), P.dma_stream("u1")]
        d_d = [P.dma_stream("dd0"), P.dma_stream("dd1")]
        d_o = P.dma_stream("o")
        P.dma("sp", P.dma_stream("c"), wts[:], T["wts"][:, :], w=[r_wts])
        nd = 0
        ny = 0
        nt_ = 0
        for e in range(8):
            for k in range(32):
                s = k % 2
                P.dma("sp", d_g[s], gst[s][:], T["wg"][e, k * 128:(k + 1) * 128, :], w=[r_gst[s]])
                P.op("act", f_act(Wg[:, k, :], gst[s][:], AF.Copy), r=[r_gst[s]], w=[r_Wg[k]])
                P.dma("sp", d_u[s], ust[s][:], T["wu"][e, k * 128:(k + 1) * 128, :], w=[r_ust[s]])
                P.op("dve", f_copy(Wu[:, k, :], ust[s][:]), r=[r_ust[s]], w=[r_Wu[k]])
            for fc in range(6):
                for qd in range(4):
                    s = nd % 2
                    nd += 1
                    P.dma("sp", d_d[s], dstg[s][:], T["wd"][e, fc * 128:(fc + 1) * 128, qd * 1024:(qd + 1) * 1024], w=[r_dstg[s]])
                    if s == 0:
                        P.op("act", f_act(Wd[:, fc, qd * 1024:(qd + 1) * 1024], dstg[s][:], AF.Copy), r=[r_dstg[s]], w=[r_Wd[fc][qd]])
                    else:
                        P.op("dve", f_copy(Wd[:, fc, qd * 1024:(qd + 1) * 1024], dstg[s][:]), r=[r_dstg[s]], w=[r_Wd[fc][qd]])
            for ch in range(nch):
                c0 = ch * CH
                P.dma("sp", d_x, xT[:], T["xgT"][e, :, c0:c0 + CH].rearrange("(kc p) s -> p kc s", p=128), w=[r_xT])
                for fh in range(2):
                    for j in range(3):
                        ft = fh * 3 + j
                        for k in range(32):
                            P.op("pe", f_mm(pG[j][:, 0:CH], Wg[:, k, ft * 128:(ft + 1) * 128], xT[:, k, :], k == 0, k == 31),
                                 r=[r_Wg[k], r_xT], w=[r_pG[j]], inc=(k == 31))
                        for k in range(32):
                            P.op("pe", f_mm(pU[j][:, 0:CH], Wu[:, k, ft * 128:(ft + 1) * 128], xT[:, k, :], k == 0, k == 31),
                                 r=[r_Wu[k], r_xT], w=[r_pU[j]], inc=(k == 31))
                        ts_ = nt_ % 2
                        nt_ += 1
                        P.op("act", f_act(tmp[ts_][:], pG[j][:, 0:CH], AF.Silu), r=[r_pG[j]], w=[r_tmp[ts_]])
                        P.op("dve", f_tt(hT[:, ft, :], tmp[ts_][:], pU[j][:, 0:CH], ALU.mult), r=[r_tmp[ts_], r_pU[j]], w=[r_hT[ft]])
                for st_ in range(nst):
                    for dg in range(8):
                        pb = ny % 2
                        ny += 1
                        for fc in range(6):
                            P.op("pe", f_mm(pD[pb][:, :], hT[:, fc, st_ * 128:(st_ + 1) * 128], Wd[:, fc, dg * 512:(dg + 1) * 512],
                                            fc == 0, fc == 5),
                                 r=[r_hT[fc], r_Wd[fc][dg // 2]], w=[r_pD[pb]], inc=(fc == 5))
                        wcol = e * (CAP // 128) + ch * nst + st_
                        if pb == 0:
                            P.op("act", f_act(ys[pb][:], pD[pb][:, :], AF.Copy, scale=wts[:, wcol:wcol + 1]),
                                 r=[r_pD[pb], r_wts], w=[r_ys[pb]])
                        else:
                            P.op("dve", f_ts(ys[pb][:], pD[pb][:, :], wts[:, wcol:wcol + 1], None, ALU.mult),
                                 r=[r_pD[pb], r_wts], w=[r_ys[pb]])
                        row = c0 + st_ * 128
                        P.dma("sp", d_o, T["y"][e, row:row + 128, dg * 512:(dg + 1) * 512], ys[pb][:], r=[r_ys[pb]])
        P.barrier()
        with nc.Block() as block:
            P.flush(block)
    return nc


def build_L4():
    nc = bass.Bass("TRN2", target_bir_lowering=False)
    T = {}
    for name, shape, dt in (("h2", [1024, D], F32), ("ya", [1024, D], BF16), ("yb", [1024, D], BF16), ("fnw", [128, D], F32)):
        T[name] = nc.dram_tensor(name, list(shape), dt, kind="ExternalInput").ap()
    T["out"] = nc.dram_tensor("out", [1024, D], F32, kind="ExternalOutput").ap()
    with ExitStack() as stack:
        P = Prog(nc, stack)
        C = Ctx(nc, stack)
        ht = [C.sb(f"ht{i}", [128, D], F32) for i in range(2)]
        ya = [C.sb(f"ya{i}", [128, D], BF16) for i in range(2)]
        yb = [C.sb(f"yb{i}", [128, D], BF16) for i in range(2)]
        t3 = C.sb("t3", [128, D], F32)
        jk = C.sb("jk", [128, D], F32)
        ot = [C.sb(f"ot{i}", [128, D], F32) for i in range(2)]
        fnw = C.sb("fnw_sb", [128, D], F32)
        stat = C.sb("stat", [128, 8], F32)
        r_ht, r_ya, r_yb, r_ot = [Res(), Res()], [Res(), Res()], [Res(), Res()], [Res(), Res()]
        r_t3, r_jk, r_st, r_fnw = Res(), Res(), Res(), Res()
        d_h = [P.dma_stream("h0"), P.dma_stream("h1")]
        d_a = [P.dma_stream("a0"), P.dma_stream("a1")]
        d_b = [P.dma_stream("b0"), P.dma_stream("b1")]
        d_o = P.dma_stream("o")
        P.dma("sp", P.dma_stream("c"), fnw[:], T["fnw"][:, :], w=[r_fnw])
        for tt in range(8):
            s = tt % 2
            rows = slice(tt * 128, (tt + 1) * 128)
            P.dma("sp", d_h[s], ht[s][:], T["h2"][rows, :], w=[r_ht[s]])
            P.dma("sp", d_a[s], ya[s][:], T["ya"][rows, :], w=[r_ya[s]])
            P.dma("sp", d_b[s], yb[s][:], T["yb"][rows, :], w=[r_yb[s]])
            P.op("dve", f_tt(t3[:], ya[s][:], yb[s][:], ALU.add), r=[r_ya[s], r_yb[s]], w=[r_t3])
            P.op("dve", f_tt(t3[:], t3[:], ht[s][:], ALU.add), r=[r_t3, r_ht[s]], w=[r_t3])
            P.op("act", f_act(jk[:], t3[:], AF.Square, accum_out=stat[:, 0:1]), r=[r_t3], w=[r_jk, r_st])
            P.op("act", f_act(stat[:, 1:2], stat[:, 0:1], AF.Sqrt, scale=1.0 / D, bias=EPS), r=[r_st], w=[r_st])
            P.op("dve", f_recip(stat[:, 2:3], stat[:, 1:2]), r=[r_st], w=[r_st])
            P.op("dve", f_stt(ot[s][:], t3[:], stat[:, 2:3], fnw[:], ALU.mult, ALU.mult), r=[r_t3, r_st, r_fnw], w=[r_ot[s]])
            P.dma("sp", d_o, T["out"][rows, :], ot[s][:], r=[r_ot[s]])
        P.barrier()
        with nc.Block() as block:
            P.flush(block)
    return nc


def build_L1():
    nc = bass.Bass("TRN2", target_bir_lowering=False)
    T = {}
    for name, shape, dt in (("x", [NB * SEQ, D], F32), ("meta", [16, D], F32), ("w_in", [D, NCOL], F32),
                            ("n1w", [128, 32], F32), ("idb", [128, 128], BF16), ("pp", [128, NPP], F32)):
        T[name] = nc.dram_tensor(name, list(shape), dt, kind="ExternalInput").ap()
    for name, shape, dt in (("fm_scr", [8, 128, NTOK], BF16), ("v_scr", [NTOK, 256], BF16), ("z_scr", [NTOK, 256], F32),
                            ("dt_scr", [NTOK, 4], F32)):
        T[name] = nc.dram_tensor(name, list(shape), dt).ap()
    T["mixT"] = nc.dram_tensor("mixT", [(NB * SEQ // 1024) * 512, 1024], BF16, kind="ExternalOutput").ap()
    T["_r_mix"] = Res()
    with ExitStack() as stack:
        P = Prog(nc, stack)
        phase1(nc, P, T, "p1")
        phase2(nc, P, T, "p2")
    return nc


OFF_K, OFF_V, OFF_Z, OFF_X, OFF_B, OFF_C, OFF_DT = 2048, 4096, 6144, 8192, 10240, 11264, 12288


def core_cols(c):
    cols = []
    cols += list(range(c * 256, c * 256 + 256))
    cols += list(range(OFF_K + c * 256, OFF_K + c * 256 + 256))
    cols += list(range(OFF_X + c * 256, OFF_X + c * 256 + 256))
    cols += list(range(OFF_B + c * 128, OFF_B + c * 128 + 128))
    cols += list(range(OFF_C + c * 128, OFF_C + c * 128 + 128))
    cols += list(range(OFF_V + c * 256, OFF_V + c * 256 + 256))
    cols += list(range(OFF_DT + c * 4, OFF_DT + c * 4 + 4))
    cols += list(range(OFF_Z + c * 256, OFF_Z + c * 256 + 256))
    return np.array(cols)


def t5_bucket_np(rel):
    n = np.maximum(rel, 0)
    nf = np.maximum(n, 1).astype(np.float32)
    large = 16 + (np.log(nf / np.float32(16)) / np.float32(np.log(128 / 16)) * np.float32(16)).astype(np.int32)
    large = np.minimum(large, 31)
    return np.where(n < 16, n, large)


def make_pp(inp, c):
    pp = np.zeros((128, NPP), np.float32)

    def put(name, arr):
        lo, hi = PP[name]
        pp[:, lo:hi] = arr

    p = np.arange(128)
    chs = [c * 256 + p, c * 256 + 128 + p, 2048 + c * 128 + p, 3072 + c * 128 + p]
    cw = inp["conv_w"][0]
    cb = inp["conv_b"][0]
    convw = np.zeros((128, 16), np.float32)
    convb = np.zeros((128, 4), np.float32)
    for j in range(4):
        for tap in range(4):
            convw[:, j * 4 + tap] = cw[tap, chs[j]]
        convb[:, j] = cb[chs[j]]
    put("convw", convw)
    put("convb", convb)
    q = np.arange(128)[:, None]
    j = np.arange(256)[None, :]
    rel = q + 128 - j
    rb = inp["rel_bias"][:, c]
    put("biasT", np.where(rel >= 0, rb[t5_bucket_np(rel)], np.float32(0)))
    put("maskT", np.where(rel >= 0, np.float32(0), np.float32(NEG)))
    put("c31", np.full((128, 1), rb[31], np.float32))
    put("alog", np.tile(inp["a_log"][0][4 * c:4 * c + 4][None], (128, 1)))
    put("dtb", np.tile(inp["dt_bias"][0][4 * c:4 * c + 4][None], (128, 1)))
    put("dsk", np.tile(inp["d_skip"][0][4 * c:4 * c + 4][None], (128, 1)))
    put("ssmw", np.tile(inp["ssm_norm_w"][0][c * 256:(c + 1) * 256][None], (128, 1)))
    put("subw", np.tile(inp["subln_w"][0][None], (128, 1)))
    for n, k in (("lq1", "lambda_q1"), ("lk1", "lambda_k1"), ("lq2", "lambda_q2"), ("lk2", "lambda_k2")):
        put(n, np.tile(inp[k][0][None], (128, 1)))
    s_ = np.arange(128)[:, None]
    l_ = np.arange(128)[None, :]
    put("tri", (s_ <= l_).astype(np.float32))
    put("mneg", np.where(l_ >= s_, np.float32(0), np.float32(NEG)))
    put("padm", (np.arange(128) >= 112).astype(np.float32)[:, None])
    put("ones", np.ones((128, 128), np.float32))
    return pp


def host_consts(inp):
    rows = []
    for r in range(8):
        rows += list(range(r * 256, r * 256 + 256)) + list(range(2048 + r * 256, 2048 + r * 256 + 256))
    return {
        "w_out_p": np.ascontiguousarray(inp["w_out"][0][np.array(rows)]),
        "n2w": np.ascontiguousarray(np.tile(inp["norm2_w"][0][None], (128, 1))),
        "wr": np.ascontiguousarray(np.concatenate([inp["router_group_w"][0], inp["router_expert_w"][0]], axis=1)),
        "rb": np.ascontiguousarray(np.tile(np.concatenate([inp["router_group_b"][0], inp["router_expert_b"][0]])[None], (128, 1))),
        "idb": np.eye(128, dtype=np.float32).astype(ml_dtypes.bfloat16),
        "io8": np.ascontiguousarray(np.tile(np.arange(8, dtype=np.float32)[None], (128, 1))),
    }


def make_inputs(inp, cores):
    x = np.ascontiguousarray(inp["x"].reshape(NB * SEQ, D))
    meta = np.ascontiguousarray(inp["meta_tokens"])
    n1w = np.ascontiguousarray(inp["norm1_w"][0].reshape(32, 128).T)
    idb = np.eye(128, dtype=np.float32).astype(ml_dtypes.bfloat16)
    maps = []
    for c in cores:
        m = {"x": x, "meta": meta, "n1w": n1w, "idb": idb,
             "w_in": np.ascontiguousarray(inp["w_in"][0][:, core_cols(c)]),
             "pp": make_pp(inp, c)}
        maps.append(m)
    return maps


def kernel(**inp):
    cores = list(range(NCORES))
    x2 = np.ascontiguousarray(inp["x"].reshape(NB * SEQ, D))
    res1 = run_bass_kernel_spmd(build_L1(), make_inputs(inp, cores), core_ids=cores)
    mixT = [np.asarray(r["mixT"]) for r in res1.results]
    hc = host_consts(inp)
    maps2 = []
    for j in cores:
        mT = np.ascontiguousarray(np.concatenate([mixT[r][j * 512:(j + 1) * 512] for r in cores], axis=0))
        maps2.append({"mT_in": mT, "w_out": hc["w_out_p"], "xres": np.ascontiguousarray(x2[j * 1024:(j + 1) * 1024]),
                      "n2w": hc["n2w"], "wr": hc["wr"], "rb": hc["rb"], "idb": hc["idb"], "io8": hc["io8"]})
    res2 = run_bass_kernel_spmd(build_L2(), maps2, core_ids=cores)
    h2 = [np.asarray(r["h2"]) for r in res2.results]
    u2 = np.concatenate([np.asarray(r["u2"]) for r in res2.results], axis=0)
    route = np.concatenate([np.asarray(r["route"]) for r in res2.results], axis=0)
    ntok = NB * SEQ
    eid = route[:, 0:2].astype(np.int64)
    wgt = route[:, 2:4]
    lists = []
    for e in range(64):
        t0_ = np.nonzero(eid[:, 0] == e)[0]
        t1_ = np.nonzero(eid[:, 1] == e)[0]
        lists.append((t0_, t1_))
    mx = max(len(a_) + len(b_) for a_, b_ in lists)
    CAP = max(128, -(-mx // 128) * 128)
    if CAP > 384:
        CAP = -(-CAP // 384) * 384
    slot = np.zeros((ntok, 2), np.int64)
    maps3 = []
    for g in cores:
        xgT = np.zeros((8, D, CAP), ml_dtypes.bfloat16)
        wts = np.zeros((8, CAP), np.float32)
        for ee in range(8):
            t0_, t1_ = lists[g * 8 + ee]
            toks = np.concatenate([t0_, t1_])
            n_ = len(toks)
            xgT[ee, :, :n_] = u2[toks].T
            wts[ee, :len(t0_)] = wgt[t0_, 0]
            wts[ee, len(t0_):n_] = wgt[t1_, 1]
            slot[t0_, 0] = np.arange(len(t0_))
            slot[t1_, 1] = len(t0_) + np.arange(len(t1_))
        wts_t = np.ascontiguousarray(wts.reshape(8, CAP // 128, 128).transpose(2, 0, 1).reshape(128, 8 * (CAP // 128)))
        maps3.append({"wg": np.ascontiguousarray(inp["expert_w_gate"][0][g * 8:(g + 1) * 8]),
                      "wu": np.ascontiguousarray(inp["expert_w_up"][0][g * 8:(g + 1) * 8]),
                      "wd": np.ascontiguousarray(inp["expert_w_down"][0][g * 8:(g + 1) * 8]),
                      "xgT": xgT, "wts": wts_t})
    res3 = run_bass_kernel_spmd(build_L3(CAP), maps3, core_ids=cores)
    yall = np.stack([np.asarray(r["y"]) for r in res3.results], axis=0).reshape(64, CAP, D)
    ya = yall[eid[:, 0], slot[:, 0]]
    yb = yall[eid[:, 1], slot[:, 1]]
    fnw = np.ascontiguousarray(np.tile(inp["final_norm_w"][None], (128, 1)))
    maps4 = [{"h2": h2[j], "ya": np.ascontiguousarray(ya[j * 1024:(j + 1) * 1024]),
              "yb": np.ascontiguousarray(yb[j * 1024:(j + 1) * 1024]), "fnw": fnw} for j in cores]
    res4 = run_bass_kernel_spmd(build_L4(), maps4, core_ids=cores)
    out = np.concatenate([np.asarray(r["out"]) for r in res4.results], axis=0)
    return out.reshape(NB, SEQ, D).astype(np.float32)
```
